# Optimizing a Trainium2 kernel written in Bass

```python
import math
import jax, jax.numpy as jnp
from jax import lax
import numpy as np

D_MODEL = 1024
BATCH = 16
SEQ = 2048
DEPTH = 1

HEAD_DIM = 64
A_Q_HEADS = 8
A_KV_HEADS = 2
B_DILATION_GROUPS = ((128, 1), (512, 4), (2048, 16))
B_HEADS_PER_GROUP = 4
B_HEADS = B_HEADS_PER_GROUP * len(B_DILATION_GROUPS)
GRID_W = 64
A_ROPE_THETA = 10000.0
PARTIAL_ROPE_THETA = 500000.0
PARTIAL_ROPE_DIMS = HEAD_DIM // 4
N_EXPERTS = 16
EC_CAPACITY_FACTOR = 2
D_FF_EXPERT = 2 * D_MODEL
Q_BLOCK = 128
LN_EPS = 1e-5
QK_NORM_EPS = 1e-6
MASK_VALUE = -1e30
DEEPNORM_ALPHA = (2.0 * DEPTH) ** 0.25
DEEPNORM_BETA = (8.0 * DEPTH) ** -0.25

A_Q_W = A_Q_HEADS * HEAD_DIM
A_KV_W = A_KV_HEADS * HEAD_DIM
B_W = B_HEADS * HEAD_DIM
B_OUT_W = B_HEADS_PER_GROUP * HEAD_DIM
IN_WIDTHS = (A_Q_W, A_KV_W, A_KV_W, B_W, B_W, B_W, D_MODEL, D_MODEL)
IN_TOTAL = sum(IN_WIDTHS)

kernel_name = "hybrid_gqa_dilated_ec_moe_encoder"


def layer_norm(x, g, b):
    xf = x.astype(jnp.float32)
    mu = jnp.mean(xf, axis=-1, keepdims=True)
    var = jnp.mean(jnp.square(xf - mu), axis=-1, keepdims=True)
    return ((xf - mu) * lax.rsqrt(var + LN_EPS) * g.astype(jnp.float32) + b.astype(jnp.float32)).astype(x.dtype)


def rms_norm(x, g):
    xf = x.astype(jnp.float32)
    y = xf * lax.rsqrt(jnp.mean(xf * xf, axis=-1, keepdims=True) + QK_NORM_EPS)
    return (y * g.astype(jnp.float32)).astype(x.dtype)


def rope_angles(pos, dim, theta):
    inv_freq = theta ** (-jnp.arange(0, dim, 2, dtype=jnp.float32) / dim)
    return pos.astype(jnp.float32)[:, None] * inv_freq[None, :]


def apply_rotary(x, ang):
    half = x.shape[-1] // 2
    cos = jnp.cos(ang)[None, :, None, :]
    sin = jnp.sin(ang)[None, :, None, :]
    x1 = x[..., :half].astype(jnp.float32)
    x2 = x[..., half:].astype(jnp.float32)
    return jnp.concatenate([x1 * cos - x2 * sin, x2 * cos + x1 * sin], axis=-1).astype(x.dtype)


def gqa_blocked(q, k, v):
    B, S, Hq, dh = q.shape
    Hkv = k.shape[2]
    G = Hq // Hkv
    nblk = S // Q_BLOCK
    scale = dh ** -0.5
    qb = q.reshape(B, nblk, Q_BLOCK, Hkv, G, dh).transpose(1, 0, 2, 3, 4, 5)

    def one_block(qblk):
        s = jnp.einsum('bqkgd,bskd->bkgqs', qblk, k, preferred_element_type=jnp.float32) * scale
        p = jax.nn.softmax(s, axis=-1).astype(v.dtype)
        return jnp.einsum('bkgqs,bskd->bqkgd', p, v)

    o = lax.map(one_block, qb)
    return o.transpose(1, 0, 2, 3, 4, 5).reshape(B, S, Hq * dh)


def dilated_band_attention(q, k, v, window, dilation):
    B, S, H, dh = q.shape
    half = window // (2 * dilation)
    n = S // dilation
    Qb = half
    nb = -(-n // Qb)
    pad_end = nb * Qb - n
    scale = dh ** -0.5

    def split(t):
        return t.reshape(B, n, dilation, H, dh).transpose(0, 2, 1, 3, 4)

    qs = jnp.pad(split(q), ((0, 0), (0, 0), (0, pad_end), (0, 0), (0, 0)))
    qs = qs.reshape(B, dilation, nb, Qb, H, dh)

    def key_blocks(t):
        tp = jnp.pad(split(t), ((0, 0), (0, 0), (Qb, pad_end + Qb), (0, 0), (0, 0)))
        tp = tp.reshape(B, dilation, nb + 2, Qb, H, dh)
        return jnp.concatenate([tp[:, :, :-2], tp[:, :, 1:-1], tp[:, :, 2:]], axis=3)

    ks = key_blocks(k)
    vs = key_blocks(v)
    a = jnp.arange(Qb)
    c = jnp.arange(3 * Qb)
    blk = jnp.arange(nb)
    rel = c[None, :] - Qb - a[:, None]
    key_idx = blk[:, None] * Qb + c[None, :] - Qb
    valid = (key_idx >= 0) & (key_idx < n)
    mask = (jnp.abs(rel) <= half)[None, :, :] & valid[:, None, :]

    s = jnp.einsum('brnqhd,brnkhd->brnhqk', qs, ks, preferred_element_type=jnp.float32) * scale
    s = jnp.where(mask[None, None, :, None, :, :], s, MASK_VALUE)
    lse = jax.nn.logsumexp(s, axis=-1)
    p = jnp.exp(s - lse[..., None]).astype(v.dtype)
    o = jnp.einsum('brnhqk,brnkhd->brnqhd', p, vs)
    o = o.reshape(B, dilation, nb * Qb, H, dh)[:, :, :n]
    o = o.transpose(0, 2, 1, 3, 4).reshape(B, S, H, dh)
    lse = lse.transpose(0, 1, 2, 4, 3).reshape(B, dilation, nb * Qb, H)[:, :, :n]
    lse = lse.transpose(0, 2, 1, 3).reshape(B, S, H)
    return o, lse


def hybrid_mixer(h, w_in, b_gate, qn_g, kn_g, w_branch_a, w_branch_b, w_out, ang_row, ang_col, ang_t):
    B, S, _ = h.shape
    proj = jnp.einsum('bsd,de->bse', h, w_in)
    offsets = []
    acc = 0
    for w in IN_WIDTHS[:-1]:
        acc += w
        offsets.append(acc)
    qa, ka, va, qb, kb, vb, ga, gb = jnp.split(proj, offsets, axis=-1)

    half_rot = HEAD_DIM // 2
    qa = rms_norm(qa.reshape(B, S, A_Q_HEADS, HEAD_DIM), qn_g)
    ka = rms_norm(ka.reshape(B, S, A_KV_HEADS, HEAD_DIM), kn_g)
    qa = jnp.concatenate([apply_rotary(qa[..., :half_rot], ang_row), apply_rotary(qa[..., half_rot:], ang_col)], axis=-1)
    ka = jnp.concatenate([apply_rotary(ka[..., :half_rot], ang_row), apply_rotary(ka[..., half_rot:], ang_col)], axis=-1)
    va = va.reshape(B, S, A_KV_HEADS, HEAD_DIM)
    ya = jnp.einsum('bse,ed->bsd', gqa_blocked(qa, ka, va), w_branch_a)

    def partial_rope(t):
        t = t.reshape(B, S, B_HEADS, HEAD_DIM)
        return jnp.concatenate([apply_rotary(t[..., :PARTIAL_ROPE_DIMS], ang_t), t[..., PARTIAL_ROPE_DIMS:]], axis=-1)
    qb = partial_rope(qb)
    kb = partial_rope(kb)
    vb = vb.reshape(B, S, B_HEADS, HEAD_DIM)
    outs = []
    lses = []
    for gi, (window, dilation) in enumerate(B_DILATION_GROUPS):
        sl = slice(gi * B_HEADS_PER_GROUP, (gi + 1) * B_HEADS_PER_GROUP)
        o, lse = dilated_band_attention(qb[:, :, sl], kb[:, :, sl], vb[:, :, sl], window, dilation)
        outs.append(o)
        lses.append(lse)
    wts = jax.nn.softmax(jnp.stack(lses, axis=0), axis=0)
    ob = jnp.einsum('gbsh,gbshd->bshd', wts.astype(vb.dtype), jnp.stack(outs, axis=0))
    yb = jnp.einsum('bse,ed->bsd', ob.reshape(B, S, B_OUT_W), w_branch_b)

    gates = jax.nn.sigmoid(jnp.concatenate([ga, gb], axis=-1).astype(jnp.float32) + b_gate.astype(jnp.float32))
    gates = gates.astype(ya.dtype)
    merged = gates[..., :D_MODEL] * ya + gates[..., D_MODEL:] * yb
    return jnp.einsum('bsd,de->bse', merged, w_out)


def expert_choice_ffn(h, w_router, w_gate, w_up, w_down):
    B, S, D = h.shape
    cap = EC_CAPACITY_FACTOR * S // N_EXPERTS
    logits = jnp.einsum('bsd,de->bse', h, w_router, preferred_element_type=jnp.float32)
    aff = jax.nn.softmax(logits, axis=-1)
    gate_vals, tok_idx = lax.top_k(aff.transpose(0, 2, 1), cap)
    xe = jax.vmap(lambda hb, ib: hb[ib])(h, tok_idx)
    g = jnp.einsum('becd,edf->becf', xe, w_gate)
    u = jnp.einsum('becd,edf->becf', xe, w_up)
    ye = jnp.einsum('becf,efd->becd', jax.nn.silu(g) * u, w_down)
    ye = ye * gate_vals[..., None].astype(ye.dtype)

    def combine(ib, yb):
        return jnp.zeros((S, D), yb.dtype).at[ib.reshape(-1)].add(yb.reshape(-1, D))

    return jax.vmap(combine)(tok_idx, ye)


def setup_inputs(seed: int = 0) -> dict:
    key = jax.random.key(seed)
    ks = jax.random.split(key, 20)
    L, D, E, F = DEPTH, D_MODEL, N_EXPERTS, D_FF_EXPERT
    nrm = lambda k, shape: jax.random.normal(k, shape, jnp.float32)
    col_scale = jnp.concatenate([
        jnp.full((w,), DEEPNORM_BETA if idx in (2, 5) else 1.0, jnp.float32)
        for idx, w in enumerate(IN_WIDTHS)])
    return {
        "x": nrm(ks[0], (BATCH, SEQ, D)),
        "ln0_g": 1.0 + 0.02 * nrm(ks[1], (D,)),
        "ln0_b": 0.02 * nrm(ks[2], (D,)),
        "w_in": nrm(ks[3], (L, D, IN_TOTAL)) * (D ** -0.5) * col_scale,
        "b_gate": 0.1 * nrm(ks[4], (L, 2 * D)),
        "qn_g": 1.0 + 0.02 * nrm(ks[5], (L, HEAD_DIM)),
        "kn_g": 1.0 + 0.02 * nrm(ks[6], (L, HEAD_DIM)),
        "w_branch_a": nrm(ks[7], (L, A_Q_W, D)) * (A_Q_W ** -0.5),
        "w_branch_b": nrm(ks[8], (L, B_OUT_W, D)) * (B_OUT_W ** -0.5),
        "w_out": nrm(ks[9], (L, D, D)) * (D ** -0.5) * DEEPNORM_BETA,
        "ln1_g": 1.0 + 0.02 * nrm(ks[10], (L, D)),
        "ln1_b": 0.02 * nrm(ks[11], (L, D)),
        "w_router": nrm(ks[12], (L, D, E)) * (D ** -0.5),
        "w_gate_e": nrm(ks[13], (L, E, D, F)) * (D ** -0.5),
        "w_up_e": nrm(ks[14], (L, E, D, F)) * (D ** -0.5),
        "w_down_e": nrm(ks[15], (L, E, F, D)) * (F ** -0.5) * DEEPNORM_BETA,
        "ln2_g": 1.0 + 0.02 * nrm(ks[16], (L, D)),
        "ln2_b": 0.02 * nrm(ks[17], (L, D)),
    }


def reference(x, ln0_g, ln0_b, w_in, b_gate, qn_g, kn_g, w_branch_a, w_branch_b, w_out,
              ln1_g, ln1_b, w_router, w_gate_e, w_up_e, w_down_e, ln2_g, ln2_b):
    S = x.shape[1]
    rows = S // GRID_W
    t = jnp.arange(S)
    row_idx = jnp.repeat(jnp.arange(rows), GRID_W)
    col_idx = jnp.tile(jnp.arange(GRID_W), rows)
    ang_row = rope_angles(row_idx, HEAD_DIM // 2, A_ROPE_THETA)
    ang_col = rope_angles(col_idx, HEAD_DIM // 2, A_ROPE_THETA)
    ang_t = rope_angles(t, PARTIAL_ROPE_DIMS, PARTIAL_ROPE_THETA)

    h = layer_norm(x, ln0_g, ln0_b)
    for l in range(DEPTH):
        mix = hybrid_mixer(h, w_in[l], b_gate[l], qn_g[l], kn_g[l], w_branch_a[l], w_branch_b[l],
                           w_out[l], ang_row, ang_col, ang_t)
        h = layer_norm(DEEPNORM_ALPHA * h + mix, ln1_g[l], ln1_b[l])
        ffn = expert_choice_ffn(h, w_router[l], w_gate_e[l], w_up_e[l], w_down_e[l])
        h = layer_norm(DEEPNORM_ALPHA * h + ffn, ln2_g[l], ln2_b[l])
    return h
```

```python
import numpy as np
import ml_dtypes
from contextlib import ExitStack
import concourse.bass as bass
import concourse.mybir as mybir
from concourse.bass_utils import run_bass_kernel_spmd

F32 = mybir.dt.float32
BF16 = mybir.dt.bfloat16
ALU = mybir.AluOpType
AF = mybir.ActivationFunctionType
AX = mybir.AxisListType

S = 2048
D = 1024
NCORE = 8
SPC = 2
NE = 16
CAP = 256
FF = 2048
ALPHA = float(2.0 ** 0.25)
LN_EPS = 1e-5
QK_EPS = 1e-6
HA = [0, 4, 1, 5, 2, 6, 3, 7]
DIL = (1, 4, 16)


def _isz(dt):
    return 4 if dt == F32 else 2


def _box(ap):
    a = ap.ap
    off = ap.offset
    isz = _isz(ap.dtype)
    sp = str(ap.space)
    if sp in ("SB", "PSUM"):
        pstep, pcnt = a[0]
        p0 = ap.base_partition()
        f0 = off - p0 * pstep
        ext = 0
        for st, ct in a[1:]:
            ext += abs(st) * (ct - 1)
        return (ap.name, p0, p0 + pcnt, f0 * isz, (f0 + ext + 1) * isz)
    ext = 0
    for st, ct in a:
        ext += abs(st) * (ct - 1)
    return (ap.name, 0, 1, off * isz, (off + ext + 1) * isz)


def _ovl(b1, b2):
    return b1[1] < b2[2] and b2[1] < b1[2] and b1[3] < b2[4] and b2[3] < b1[4]


def _cov(b1, b2):
    return b1[1] <= b2[1] and b1[2] >= b2[2] and b1[3] <= b2[3] and b1[4] >= b2[4]


class Op:
    __slots__ = ("eng", "fn", "deps", "signal", "sigval", "is_dma", "dsem", "dval", "idx", "prev_dma")

    def __init__(self, eng, fn):
        self.eng = eng
        self.fn = fn
        self.deps = []
        self.signal = False
        self.sigval = None
        self.is_dma = False
        self.dsem = None
        self.dval = None
        self.idx = None
        self.prev_dma = None


class Prog:
    ENGS = ("pe", "act", "dve", "pool", "sp")
    NDSEM = 8

    def __init__(self, nc):
        self.nc = nc
        self.ops = {e: [] for e in self.ENGS}
        self.acc = {}
        self.dma_count = {e: 0 for e in self.ENGS}
        self.dma_last = {}
        self.dma_slot_count = {}
        self.skip = False

    def _deps_for(self, op, reads, writes):
        deps = []
        rb_ = [_box(ap) for ap in reads]
        wb_ = [_box(ap) for ap in writes]
        for b in rb_:
            t = self.acc.get(b[0])
            if t is None:
                t = self.acc[b[0]] = {"w": [], "r": {}}
            for wb, wop in t["w"]:
                if _ovl(wb, b):
                    deps.append(wop)
        for b in wb_:
            t = self.acc.get(b[0])
            if t is None:
                t = self.acc[b[0]] = {"w": [], "r": {}}
            for wb, wop in t["w"]:
                if _ovl(wb, b):
                    deps.append(wop)
            for k, rop in t["r"].items():
                if _ovl(k[1], b):
                    deps.append(rop)
        ekey = ("dma", id(op)) if op.is_dma else op.eng
        for b in rb_:
            self.acc[b[0]]["r"][(ekey, b)] = op
        for b in wb_:
            t = self.acc[b[0]]
            t["w"] = [(wb, wop) for wb, wop in t["w"] if not _cov(b, wb)]
            t["w"].append((b, op))
            t["r"] = {k: v for k, v in t["r"].items() if not _cov(b, k[1])}
        best = {}
        out = []
        seen = set()
        for d in deps:
            if d is op:
                continue
            if d.is_dma:
                if id(d) not in seen:
                    seen.add(id(d))
                    out.append(d)
            else:
                if d.eng == "pe" and op.eng == "pe" and not op.is_dma:
                    continue
                cur = best.get(d.eng)
                if cur is None or d.idx > cur.idx:
                    best[d.eng] = d
        out.extend(best.values())
        op.deps = out
        for d in out:
            d.signal = True

    def op(self, eng, fn, reads=(), writes=()):
        if self.skip:
            return None
        o = Op(eng, fn)
        o.idx = len(self.ops[eng])
        self._deps_for(o, reads, writes)
        self.ops[eng].append(o)
        return o

    def dma(self, q, out, in_):
        if self.skip:
            return None
        o = Op(q, lambda e: e.dma_start(out=out, in_=in_))
        o.is_dma = True
        o.idx = len(self.ops[q])
        slot = self.dma_count[q] % self.NDSEM
        self.dma_count[q] += 1
        o.dsem = (q, slot)
        c = self.dma_slot_count.get((q, slot), 0) + 1
        self.dma_slot_count[(q, slot)] = c
        o.dval = 16 * c
        o.prev_dma = self.dma_last.get((q, slot))
        self.dma_last[(q, slot)] = o
        self._deps_for(o, [in_], [out])
        self.ops[q].append(o)
        return o

    def emit(self):
        nc = self.nc
        with ExitStack() as es:
            esem = {e: es.enter_context(nc.semaphore("s_" + e)) for e in self.ENGS}
            dsem = {}
            for q in self.ENGS:
                for s in range(min(self.NDSEM, self.dma_count[q])):
                    dsem[(q, s)] = es.enter_context(nc.semaphore("d_%s_%d" % (q, s)))
            for e in self.ENGS:
                c = 0
                for o in self.ops[e]:
                    if o.signal and not o.is_dma:
                        c += 1
                        o.sigval = c
            block = es.enter_context(nc.Block())

            def run(ename, eng):
                have = {}

                def wait(key, sem, val):
                    if have.get(key, 0) >= val:
                        return
                    eng.wait_ge(sem, val)
                    have[key] = val

                for o in self.ops[ename]:
                    for d in o.deps:
                        if d.is_dma:
                            wait(d.dsem, dsem[d.dsem], d.dval)
                        else:
                            wait(d.eng, esem[d.eng], d.sigval)
                    if o.is_dma:
                        if o.prev_dma is not None:
                            wait(o.dsem, dsem[o.dsem], o.prev_dma.dval)
                        o.fn(eng).then_inc(dsem[o.dsem], 16)
                    else:
                        ins = o.fn(eng)
                        if o.signal:
                            ins.then_inc(esem[ename], 1)
                if ename == "sp":
                    for key, o in self.dma_last.items():
                        wait(key, dsem[key], o.dval)

            @block.tensor
            def _(e):
                run("pe", e)

            @block.scalar
            def _(e):
                run("act", e)

            @block.vector
            def _(e):
                run("dve", e)

            @block.gpsimd
            def _(e):
                run("pool", e)

            @block.sync
            def _(e):
                run("sp", e)


def _aps(*xs):
    return [x for x in xs if not isinstance(x, (int, float)) and x is not None]


class K:
    def __init__(self, nc, P, big, total_bytes):
        self.nc = nc
        self.P = P
        self.big = big
        self.total = total_bytes
        self.off = 0

    def alloc(self, shape, dt):
        n = 1
        for s_ in shape:
            n *= s_
        nb = n * _isz(dt)
        nb_al = (nb + 63) // 64 * 64
        o = self.off
        self.off += nb_al
        assert self.off <= self.total, ("SBUF overflow", self.off, self.total)
        v = self.big[:, o // 2:(o + nb) // 2]
        if dt == F32:
            v = v.bitcast(F32)
        if len(shape) == 1:
            return v
        names = "abcdef"[:len(shape)]
        kw = {names[i]: shape[i] for i in range(len(shape) - 1)}
        return v.rearrange("p (" + " ".join(names) + ") -> p " + " ".join(names), **kw)

    def mark(self):
        return self.off

    def release(self, m):
        self.off = m

    def mm(self, out, lhsT, rhs, start=True, stop=True):
        self.P.op("pe", lambda e: e.matmul(out, lhsT=lhsT, rhs=rhs, start=start, stop=stop), [lhsT, rhs], [out])

    def tr(self, out, in_, ident):
        self.P.op("pe", lambda e: e.transpose(out, in_, ident), [in_, ident], [out])

    def act(self, out, in_, func, bias=0.0, scale=1.0):
        self.P.op("act", lambda e: e.activation(out=out, in_=in_, func=func, bias=bias, scale=scale),
                  _aps(in_, bias, scale), [out])

    def copy(self, eng, out, in_):
        if eng == "act":
            self.P.op("act", lambda e: e.copy(out=out, in_=in_), [in_], [out])
        else:
            self.P.op(eng, lambda e: e.tensor_copy(out=out, in_=in_), [in_], [out])

    def tt(self, eng, out, a, b, op):
        self.P.op(eng, lambda e: e.tensor_tensor(out=out, in0=a, in1=b, op=op), [a, b], [out])

    def ts(self, eng, out, a, s1, s2, op0, op1=None, accum=None):
        if op1 is None:
            self.P.op(eng, lambda e: e.tensor_scalar(out=out, in0=a, scalar1=s1, scalar2=None, op0=op0),
                      _aps(a, s1), [out])
        elif accum is None:
            self.P.op(eng, lambda e: e.tensor_scalar(out=out, in0=a, scalar1=s1, scalar2=s2, op0=op0, op1=op1),
                      _aps(a, s1, s2), [out])
        else:
            self.P.op(eng, lambda e: e.tensor_scalar(out=out, in0=a, scalar1=s1, scalar2=s2, op0=op0, op1=op1,
                                                     accum_out=accum), _aps(a, s1, s2), [out, accum])

    def stt(self, eng, out, in0, scalar, in1, op0, op1):
        self.P.op(eng, lambda e: e.scalar_tensor_tensor(out=out, in0=in0, scalar=scalar, in1=in1, op0=op0, op1=op1),
                  _aps(in0, scalar, in1), [out])

    def recip(self, out, in_):
        self.P.op("dve", lambda e: e.reciprocal(out=out, in_=in_), [in_], [out])

    def memset(self, eng, out, val):
        self.P.op(eng, lambda e: e.memset(out, val), [], [out])

    def dma(self, q, out, in_):
        self.P.dma(q, out, in_)


def _partnerA(d):
    return d + 16 if (d % 32) < 16 else d - 16


def _partnerB(d):
    if d < 8:
        return d + 8
    if d < 16:
        return d - 8
    return d


def _host_consts():
    f32 = np.float32
    t = np.arange(S)
    invA = (f32(10000.0) ** (-(np.arange(0, 32, 2, dtype=f32)) / f32(32))).astype(f32)
    ang_row = ((t // 64).astype(f32)[:, None] * invA[None, :]).astype(f32)
    ang_col = ((t % 64).astype(f32)[:, None] * invA[None, :]).astype(f32)
    invT = (f32(500000.0) ** (-(np.arange(0, 16, 2, dtype=f32)) / f32(16))).astype(f32)
    ang_t = (t.astype(f32)[:, None] * invT[None, :]).astype(f32)
    tabA = np.zeros((128, 2, S), f32)
    for p in range(128):
        dh = p % 64
        j = dh % 16
        x1 = (dh % 32) < 16
        ang = (ang_row if dh < 32 else ang_col)[:, j].astype(np.float64)
        tabA[p, 0] = np.cos(ang)
        tabA[p, 1] = -np.sin(ang) if x1 else np.sin(ang)
    tabB = np.zeros((3, 128, 2, S), f32)
    for g, r in enumerate(DIL):
        n = S // r
        tt_ = np.arange(S)
        perm = (tt_ % n) * r + (tt_ // n)
        for p in range(128):
            dh = p % 64
            if dh < 16:
                j = dh % 8
                ang = ang_t[perm, j].astype(np.float64)
                tabB[g, p, 0] = np.cos(ang)
                tabB[g, p, 1] = -np.sin(ang) if dh < 8 else np.sin(ang)
            else:
                tabB[g, p, 0] = 1.0
    c = {}
    c["tabA"] = tabA
    c["tabB"] = tabB
    c["ident_bf"] = np.eye(128, dtype=f32).astype(ml_dtypes.bfloat16)
    c["ident_f"] = np.eye(128, dtype=f32)
    k = np.arange(128)
    c["tri_bf"] = (k[:, None] < k[None, :]).astype(f32).astype(ml_dtypes.bfloat16)
    c["ones_bf"] = np.ones((128, 128), f32).astype(ml_dtypes.bfloat16)
    c["blk_f"] = ((k[:, None] // 64) == (k[None, :] // 64)).astype(f32)
    c["iota256"] = np.broadcast_to(np.arange(256, dtype=f32)[None, :], (128, 256)).copy()
    c["iota2048"] = np.broadcast_to(np.arange(2048, dtype=f32)[None, :], (128, 2048)).copy()
    q = np.arange(128)
    band = np.zeros((128, 3, 128), f32)
    band[:, 0, :] = ((k[:, None] - q[None, :]) >= 64)
    band[:, 1, :] = (np.abs(k[:, None] - q[None, :]) <= 64)
    band[:, 2, :] = ((q[None, :] - k[:, None]) >= 64)
    c["band"] = band.reshape(128, 384).astype(ml_dtypes.bfloat16)
    rc0 = np.zeros((128, 16, 16, 2), f32)
    rc0[:, :, :, 0] = np.arange(16, dtype=f32)[None, :, None]
    rc0[:, :, :, 1] = np.arange(128, dtype=f32)[:, None, None]
    c["rc0"] = rc0.astype(ml_dtypes.bfloat16)
    return c


def _host_weights(inp):
    w_in = inp["w_in"][0]
    o_qA, o_kA, o_vA, o_qB, o_kB, o_vB, o_gA = 0, 512, 640, 768, 1536, 2304, 3072
    cols = []
    for h in HA:
        cols += [o_qA + h * 64 + d for d in range(64)]
    for kv in range(2):
        cols += [o_kA + kv * 64 + d for d in range(64)]
    for h in HA:
        cols += [o_qA + h * 64 + _partnerA(d) for d in range(64)]
    for kv in range(2):
        cols += [o_kA + kv * 64 + _partnerA(d) for d in range(64)]
    cols += [o_vA + i for i in range(128)]
    wA = np.ascontiguousarray(w_in[:, cols])
    wB = np.zeros((6, D, 640), np.float32)
    for c in range(2):
        for g in range(3):
            hs = [4 * g + 2 * c, 4 * g + 2 * c + 1]
            cc = []
            for h in hs:
                cc += [o_qB + h * 64 + d for d in range(64)]
            for h in hs:
                cc += [o_kB + h * 64 + d for d in range(64)]
            for h in hs:
                cc += [o_qB + h * 64 + _partnerB(d) for d in range(64)]
            for h in hs:
                cc += [o_kB + h * 64 + _partnerB(d) for d in range(64)]
            for h in hs:
                cc += [o_vB + h * 64 + d for d in range(64)]
            wB[c * 3 + g] = w_in[:, cc]
    wG = np.ascontiguousarray(w_in[:, o_gA:o_gA + 2048])
    qn = inp["qn_g"][0]
    kn = inp["kn_g"][0]
    gvec = np.zeros((128, 4), np.float32)
    for p in range(128):
        d = p % 64
        gvec[p, 0] = qn[d]
        gvec[p, 1] = qn[_partnerA(d)]
        gvec[p, 2] = kn[d]
        gvec[p, 3] = kn[_partnerA(d)]
    bg = np.ascontiguousarray(inp["b_gate"][0].reshape(16, 128).T)
    rows = []
    for h in HA:
        rows += [h * 64 + d for d in range(64)]
    wa = np.ascontiguousarray(inp["w_branch_a"][0][rows, :])
    lnp = np.stack([inp["ln0_g"], inp["ln0_b"], inp["ln1_g"][0], inp["ln1_b"][0], inp["ln2_g"][0], inp["ln2_b"][0]])
    lnp = np.ascontiguousarray(np.broadcast_to(lnp.reshape(3, 1, 2, D), (3, 128, 2, D))).astype(np.float32)
    return dict(wA=wA, wB=wB, wG=wG, gvec=gvec, bgate=bg, wa=wa, wb=np.ascontiguousarray(inp["w_branch_b"][0]),
                wo=np.ascontiguousarray(inp["w_out"][0]), lnp=lnp, wr=np.ascontiguousarray(inp["w_router"][0]),
                wg_e=inp["w_gate_e"][0], wu_e=inp["w_up_e"][0], wd_e=inp["w_down_e"][0])


def build(debug=False, stage=7, only=None):
    stages = set(range(1, stage + 1)) if only is None else set(only)
    nc = bass.Bass("TRN2", target_bir_lowering=False)

    def din(name, shape, dt=F32):
        return nc.dram_tensor(name, list(shape), dt, kind="ExternalInput").ap()

    x_d = din("x", [SPC, S, D])
    wA_d = din("wA", [D, 1408])
    wB_d = din("wB", [6, D, 640])
    wG_d = din("wG", [D, 2048])
    gvec_d = din("gvec", [128, 4])
    bgate_d = din("bgate", [128, 16])
    wa_d = din("wa", [512, D])
    wb_d = din("wb", [256, D])
    wo_d = din("wo", [D, D])
    lnp_d = din("lnp", [3, 128, 2, D])
    wr_d = din("wr", [D, NE])
    wg_d = din("wg_e", [NE, D, FF])
    wu_d = din("wu_e", [NE, D, FF])
    wd_d = din("wd_e", [NE, FF, D])
    tabA_d = din("tabA", [128, 2, S])
    tabB_d = din("tabB", [3, 128, 2, S])
    identbf_d = din("ident_bf", [128, 128], BF16)
    identf_d = din("ident_f", [128, 128])
    tri_d = din("tri_bf", [128, 128], BF16)
    ones_d = din("ones_bf", [128, 128], BF16)
    blk_d = din("blk_f", [128, 128])
    iota256_d = din("iota256", [128, 256])
    iota2048_d = din("iota2048", [128, 2048])
    band_d = din("band", [128, 384], BF16)
    rc0_d = din("rc0", [128, 16, 16, 2], BF16)
    out_d = nc.dram_tensor("out", [SPC, S, D], F32, kind="ExternalOutput").ap()
    skind = "ExternalOutput" if debug else "Internal"
    h0_scr = nc.dram_tensor("h0_scr", [SPC, S, D], F32, kind=skind).ap()
    h1_scr = nc.dram_tensor("h1_scr", [SPC, S, D], F32, kind=skind).ap()
    ye_scr = nc.dram_tensor("ye_scr", [NE, SPC, 2, 128, D], BF16, kind="Internal").ap()
    if debug:
        aff_dbg = nc.dram_tensor("aff_dbg", [128, SPC, 16, NE], F32, kind="ExternalOutput").ap()
        idx_dbg = nc.dram_tensor("idx_dbg", [128, NE, 4], F32, kind="ExternalOutput").ap()
        gate_dbg = nc.dram_tensor("gate_dbg", [128, NE, 4], F32, kind="ExternalOutput").ap()

    TOTAL = 207 * 1024
    with ExitStack() as es:
        big = es.enter_context(nc.sbuf_tensor("big", [128, TOTAL // 2], BF16))
        pp = [es.enter_context(nc.psum_tensor("pp%d" % i, [128, 1024], F32)) for i in range(4)]
        bank = []
        for i in range(4):
            bank.append(pp[i][:, 0:512])
            bank.append(pp[i][:, 512:1024])
        P = Prog(nc)
        k = K(nc, P, big, TOTAL)

        ident_bf = k.alloc([128], BF16)
        ident_f = k.alloc([128], F32)
        tri_bf = k.alloc([128], BF16)
        ones_bf = k.alloc([128], BF16)
        blk_f = k.alloc([128], F32)
        iota256 = k.alloc([256], F32)
        band = k.alloc([384], BF16)
        gvec = k.alloc([4], F32)
        bgate = k.alloc([16], F32)
        wr = k.alloc([8, NE], F32)
        for dst, src in ((ident_bf, identbf_d), (ident_f, identf_d), (tri_bf, tri_d), (ones_bf, ones_d),
                         (blk_f, blk_d), (iota256, iota256_d), (band, band_d), (gvec, gvec_d), (bgate, bgate_d)):
            k.dma("sp", dst, src)
        k.dma("sp", wr, wr_d.rearrange("(c p) e -> p c e", p=128))
        h1b_off = k.mark()
        h1b = k.alloc([SPC, 16, D], BF16)
        aff = k.alloc([SPC, 16, NE], F32)
        base_mark = k.mark()

        def layer_norm(src, lnp, dst, st, mv, sc):
            for j in range(2):
                P.op("dve", (lambda o, i: (lambda e: e.bn_stats(out=o, in_=i)))(st[:, j, :], src[:, j * 512:(j + 1) * 512]),
                     [src[:, j * 512:(j + 1) * 512]], [st[:, j, :]])
            P.op("dve", lambda e: e.bn_aggr(out=mv, in_=st.rearrange("p a b -> p (a b)")), [st], [mv])
            k.act(sc[:, 0:1], mv[:, 1:2], AF.Sqrt, bias=LN_EPS, scale=1.0)
            k.recip(sc[:, 0:1], sc[:, 0:1])
            k.stt("dve", sc[:, 1:2], mv[:, 0:1], -1.0, sc[:, 0:1], ALU.mult, ALU.mult)
            k.act(dst, src, AF.Identity, bias=sc[:, 1:2], scale=sc[:, 0:1])
            k.tt("dve", dst, dst, lnp[:, 0, :], ALU.mult)
            k.tt("dve", dst, dst, lnp[:, 1, :], ALU.add)

        for s in range(SPC):
            k.release(base_mark)
            h0T = k.alloc([8, S], BF16)
            attnAT = k.alloc([4, S], BF16)
            obT = k.alloc([2, S], BF16)
            mixer_mark = k.mark()

            P.skip = 1 not in stages
            lnp = k.alloc([2, D], F32)
            k.dma("sp", lnp, lnp_d[0])
            xb = [k.alloc([D], F32) for _ in range(2)]
            hb = [k.alloc([D], F32) for _ in range(2)]
            hbb = [k.alloc([D], BF16) for _ in range(2)]
            st = [k.alloc([2, 6], F32) for _ in range(2)]
            mv = [k.alloc([2], F32) for _ in range(2)]
            sc = [k.alloc([2], F32) for _ in range(2)]
            for tc in range(16):
                i = tc % 2
                k.dma("sp", xb[i], x_d[s, tc * 128:(tc + 1) * 128, :])
                layer_norm(xb[i], lnp, hb[i], st[i], mv[i], sc[i])
                k.dma("sp", h0_scr[s, tc * 128:(tc + 1) * 128, :], hb[i])
                k.copy("act", hbb[i], hb[i])
                pb = pp[tc % 2][:, 0:512].bitcast(BF16)
                for dc in range(8):
                    k.tr(pb[:, dc * 128:(dc + 1) * 128], hbb[i][:, dc * 128:(dc + 1) * 128], ident_bf)
                k.copy("dve" if tc % 2 == 0 else "act", h0T[:, :, tc * 128:(tc + 1) * 128],
                       pb.rearrange("p (a b) -> p a b", a=8))
            k.release(mixer_mark)

            P.skip = 2 not in stages
            wA = k.alloc([8, 1408], BF16)
            wA_v = wA_d.rearrange("(c p) n -> p c n", p=128)
            k.dma("pool", wA[:, :, 0:704], wA_v[:, :, 0:704])
            k.dma("pool", wA[:, :, 704:1408], wA_v[:, :, 704:1408])
            qAT = k.alloc([4, S], BF16)
            kAT = k.alloc([S], BF16)
            VL = k.alloc([16, 128], BF16)
            VU = k.alloc([16, 128], BF16)
            k.memset("pool", VL[:, :, 64:128], 1.0)
            k.memset("pool", VU[:, :, 0:64], 1.0)
            tab = [k.alloc([2, 512], F32) for _ in range(2)]
            sq = [k.alloc([512], F32) for _ in range(2)]
            rs = [k.alloc([512], F32) for _ in range(2)]
            ta = [k.alloc([512], F32) for _ in range(2)]
            tb_ = [k.alloc([512], F32) for _ in range(2)]
            it = 0
            for tb in range(4):
                tsl = slice(tb * 512, (tb + 1) * 512)
                tbuf = tab[tb % 2]
                k.dma("sp", tbuf, tabA_d[:, :, tsl])
                for ch in range(5):
                    j = it % 2
                    it += 1
                    ps_m, ps_p, ps_ss = bank[2 * j], bank[2 * j + 1], bank[4 + j]
                    for dc in range(8):
                        k.mm(ps_m, wA[:, dc, ch * 128:(ch + 1) * 128], h0T[:, dc, tsl], dc == 0, dc == 7)
                    for dc in range(8):
                        k.mm(ps_p, wA[:, dc, 640 + ch * 128:640 + (ch + 1) * 128], h0T[:, dc, tsl], dc == 0, dc == 7)
                    k.act(sq[j], ps_m, AF.Square)
                    k.mm(ps_ss, blk_f, sq[j], True, True)
                    k.act(rs[j], ps_ss, AF.Sqrt, bias=QK_EPS, scale=1.0 / 64)
                    k.recip(rs[j], rs[j])
                    gi = 0 if ch < 4 else 2
                    k.stt("dve", ta[j], ps_m, gvec[:, gi:gi + 1], tbuf[:, 0, :], ALU.mult, ALU.mult)
                    k.stt("dve", tb_[j], ps_p, gvec[:, gi + 1:gi + 2], tbuf[:, 1, :], ALU.mult, ALU.mult)
                    k.tt("pool", ta[j], ta[j], tb_[j], ALU.add)
                    dst = qAT[:, ch, tsl] if ch < 4 else kAT[:, tsl]
                    k.tt("dve", dst, ta[j], rs[j], ALU.mult)
                for tcl in range(4):
                    tc = tb * 4 + tcl
                    ps_v = bank[6 + tcl % 2]
                    for dc in range(8):
                        k.mm(ps_v[:, 0:128], h0T[:, dc, tc * 128:(tc + 1) * 128], wA[:, dc, 1280:1408], dc == 0, dc == 7)
                    k.copy("act", VL[:, tc, 0:64], ps_v[:, 0:64])
                    k.copy("act", VU[:, tc, 64:128], ps_v[:, 64:128])
            pt = [k.alloc([512], BF16) for _ in range(3)]
            rc = [k.alloc([512], F32)] * 2
            steps = [(c, qb, hf, kc) for c in range(4) for qb in range(4) for hf in range(2) for kc in range(16)]

            def a_score(i):
                c, qb, hf, kc = steps[i]
                rows = slice(64 * hf, 64 * hf + 64)
                k.mm(bank[i % 3], kAT[rows, kc * 128:(kc + 1) * 128], qAT[rows, c, qb * 512:(qb + 1) * 512], True, True)

            a_score(0)
            for i in range(len(steps)):
                c, qb, hf, kc = steps[i]
                if i + 1 < len(steps):
                    a_score(i + 1)
                k.act(pt[i % 3], bank[i % 3], AF.Exp, scale=0.125)
                g_ = i // 16
                ps_o = bank[4 + g_ % 4]
                k.mm(ps_o, (VL if hf == 0 else VU)[:, kc, :], pt[i % 3], kc == 0, kc == 15)
                if kc == 15:
                    qsl = slice(qb * 512, (qb + 1) * 512)
                    r_ = rc[g_ % 2]
                    if hf == 0:
                        k.recip(r_[0:64, :], ps_o[64:128, :])
                        k.tt("dve", attnAT[0:64, c, qsl], ps_o[0:64, :], r_[0:64, :], ALU.mult)
                    else:
                        k.recip(r_[64:128, :], ps_o[0:64, :])
                        k.tt("dve", attnAT[64:128, c, qsl], ps_o[64:128, :], r_[64:128, :], ALU.mult)
            k.release(mixer_mark)

            P.skip = 3 not in stages
            accB = k.alloc([2, S], F32)
            wBs = k.alloc([8, 640], BF16)
            qBT = k.alloc([S], BF16)
            kBT = k.alloc([S], BF16)
            VBL = k.alloc([16, 128], BF16)
            VBU = k.alloc([16, 128], BF16)
            k.memset("pool", VBL[:, :, 64:128], 1.0)
            k.memset("pool", VBU[:, :, 0:64], 1.0)
            tab = [k.alloc([2, 512], F32) for _ in range(2)]
            ta = [k.alloc([512], F32) for _ in range(2)]
            tb_ = [k.alloc([512], F32) for _ in range(2)]
            eb = [k.alloc([384], BF16) for _ in range(3)]
            ptb = [k.alloc([384], BF16) for _ in range(3)]
            rcb = [k.alloc([512], F32) for _ in range(2)]
            it = 0
            for c in range(2):
                for g in range(3):
                    r = DIL[g]
                    n = S // r
                    ncc = n // 128
                    k.dma("pool", wBs, wB_d[c * 3 + g].rearrange("(c p) n -> p c n", p=128))

                    def hv(dc, p0, cnt):
                        v = h0T[:, dc, :].rearrange("p (m r) -> p r m", r=r)
                        if cnt <= n:
                            rho, m0 = p0 // n, p0 % n
                            return v[:, rho, m0:m0 + cnt]
                        rho0 = p0 // n
                        return v[:, rho0:rho0 + cnt // n, :]

                    for jb in range(4):
                        tsl = slice(jb * 512, (jb + 1) * 512)
                        tbuf = tab[it % 2]
                        k.dma("sp", tbuf, tabB_d[g][:, :, tsl])
                        for ch in range(2):
                            j = it % 2
                            it += 1
                            ps_m, ps_p = bank[2 * j], bank[2 * j + 1]
                            if n >= 512:
                                om, op_ = ps_m, ps_p
                            else:
                                om = ps_m.rearrange("p (a b) -> p a b", a=512 // n)
                                op_ = ps_p.rearrange("p (a b) -> p a b", a=512 // n)
                            for dc in range(8):
                                k.mm(om, wBs[:, dc, ch * 128:(ch + 1) * 128], hv(dc, jb * 512, 512), dc == 0, dc == 7)
                            for dc in range(8):
                                k.mm(op_, wBs[:, dc, 256 + ch * 128:256 + (ch + 1) * 128], hv(dc, jb * 512, 512), dc == 0, dc == 7)
                            k.tt("dve", ta[j], ps_m, tbuf[:, 0, :], ALU.mult)
                            k.tt("dve", tb_[j], ps_p, tbuf[:, 1, :], ALU.mult)
                            k.tt("pool", (qBT if ch == 0 else kBT)[:, tsl], ta[j], tb_[j], ALU.add)
                        for l in range(4):
                            kc = jb * 4 + l
                            ps_v = bank[6 + l % 2]
                            for dc in range(8):
                                k.mm(ps_v[:, 0:128], hv(dc, kc * 128, 128), wBs[:, dc, 512:640], dc == 0, dc == 7)
                            k.copy("act", VBL[:, kc, 0:64], ps_v[:, 0:64])
                            k.copy("act", VBU[:, kc, 64:128], ps_v[:, 64:128])
                    bsteps = [(hf, rho, a) for hf in range(2) for rho in range(r) for a in range(ncc)]

                    def b_score(i):
                        hf, rho, a = bsteps[i]
                        rows = slice(64 * hf, 64 * hf + 64)
                        qc = rho * ncc + a
                        ps_s = bank[i % 3]
                        for jj in range(3):
                            jn = a - 1 + jj
                            if 0 <= jn < ncc:
                                kc = rho * ncc + jn
                                k.mm(ps_s[:, jj * 128:(jj + 1) * 128], kBT[rows, kc * 128:(kc + 1) * 128],
                                     qBT[rows, qc * 128:(qc + 1) * 128], True, True)

                    b_score(0)
                    for i in range(len(bsteps)):
                        hf, rho, a = bsteps[i]
                        if i + 1 < len(bsteps):
                            b_score(i + 1)
                        jjs = [jj for jj in range(3) if 0 <= a - 1 + jj < ncc]
                        lo, hi = jjs[0] * 128, (jjs[-1] + 1) * 128
                        k.act(eb[i % 3][:, lo:hi], bank[i % 3][:, lo:hi], AF.Exp, scale=0.125)
                        k.tt("pool", ptb[i % 3][:, lo:hi], eb[i % 3][:, lo:hi], band[:, lo:hi], ALU.mult)
                        ps_o = bank[4 + i % 4]
                        for jj in jjs:
                            kc = rho * ncc + a - 1 + jj
                            k.mm(ps_o[:, 0:128], (VBL if hf == 0 else VBU)[:, kc, :], ptb[i % 3][:, jj * 128:(jj + 1) * 128],
                                 jj == jjs[0], jj == jjs[-1])
                        accv = accB[:, hf, :].rearrange("p (m r) -> p r m", r=r)[:, rho, a * 128:(a + 1) * 128]
                        if g == 0:
                            k.copy("dve", accv, ps_o[:, 0:128])
                        else:
                            k.tt("dve", accv, ps_o[:, 0:128], accv, ALU.add)
                for qb in range(4):
                    qsl = slice(qb * 512, (qb + 1) * 512)
                    r_ = rcb[qb % 2]
                    k.recip(r_[0:64, :], accB[64:128, 0, qsl])
                    k.tt("dve", obT[0:64, c, qsl], accB[0:64, 0, qsl], r_[0:64, :], ALU.mult)
                    k.recip(r_[64:128, :], accB[0:64, 1, qsl])
                    k.tt("dve", obT[64:128, c, qsl], accB[64:128, 1, qsl], r_[64:128, :], ALU.mult)
            k.release(mixer_mark)

            P.skip = 4 not in stages
            mergedT = k.alloc([8, S], BF16)
            p3_mark = k.mark()
            wgs = [k.alloc([8, 256], BF16) for _ in range(2)]
            was = [k.alloc([4, 128], BF16) for _ in range(2)]
            wbs = [k.alloc([2, 128], BF16) for _ in range(2)]
            gA = [k.alloc([512], F32) for _ in range(2)]
            gB = [k.alloc([512], F32) for _ in range(2)]
            m1 = [k.alloc([512], F32) for _ in range(2)]
            m2 = [k.alloc([512], F32) for _ in range(2)]
            wG_v = wG_d.rearrange("(c p) n -> p c n", p=128)
            wa_v = wa_d.rearrange("(c p) n -> p c n", p=128)
            wb_v = wb_d.rearrange("(c p) n -> p c n", p=128)
            it = 0
            for dcp in range(8):
                i = dcp % 2
                dsl = slice(dcp * 128, (dcp + 1) * 128)
                k.dma("pool", wgs[i][:, :, 0:128], wG_v[:, :, dsl])
                k.dma("pool", wgs[i][:, :, 128:256], wG_v[:, :, 1024 + dcp * 128:1024 + (dcp + 1) * 128])
                k.dma("pool", was[i], wa_v[:, :, dsl])
                k.dma("pool", wbs[i], wb_v[:, :, dsl])
                for tb in range(4):
                    tsl = slice(tb * 512, (tb + 1) * 512)
                    j = it % 2
                    it += 1
                    ps_ga, ps_gb, ps_ya, ps_yb = bank[4 * j], bank[4 * j + 1], bank[4 * j + 2], bank[4 * j + 3]
                    for dc in range(8):
                        k.mm(ps_ga, wgs[i][:, dc, 0:128], h0T[:, dc, tsl], dc == 0, dc == 7)
                    for dc in range(8):
                        k.mm(ps_gb, wgs[i][:, dc, 128:256], h0T[:, dc, tsl], dc == 0, dc == 7)
                    for c in range(4):
                        k.mm(ps_ya, was[i][:, c, :], attnAT[:, c, tsl], c == 0, c == 3)
                    for c in range(2):
                        k.mm(ps_yb, wbs[i][:, c, :], obT[:, c, tsl], c == 0, c == 1)
                    k.act(gA[j], ps_ga, AF.Sigmoid, bias=bgate[:, dcp:dcp + 1])
                    k.act(gB[j], ps_gb, AF.Sigmoid, bias=bgate[:, 8 + dcp:9 + dcp])
                    k.tt("dve", m1[j], gA[j], ps_ya, ALU.mult)
                    k.tt("dve", m2[j], gB[j], ps_yb, ALU.mult)
                    k.tt("pool", mergedT[:, dcp, tsl], m1[j], m2[j], ALU.add)
            k.release(p3_mark)

            wo = k.alloc([8, D], BF16)
            wo_v = wo_d.rearrange("(c p) n -> p c n", p=128)
            k.dma("pool", wo[:, :, 0:512], wo_v[:, :, 0:512])
            k.dma("pool", wo[:, :, 512:1024], wo_v[:, :, 512:1024])
            lnp = k.alloc([2, D], F32)
            k.dma("sp", lnp, lnp_d[1])
            h0t = [k.alloc([D], F32) for _ in range(2)]
            yb_ = [k.alloc([D], F32)] * 2
            h1t = [k.alloc([D], F32) for _ in range(2)]
            h1T = [k.alloc([8, 128], F32)] * 2
            st = [k.alloc([2, 6], F32) for _ in range(2)]
            mv = [k.alloc([2], F32) for _ in range(2)]
            sc = [k.alloc([2], F32) for _ in range(2)]
            ex = [k.alloc([NE], F32) for _ in range(2)]
            sm = [k.alloc([2], F32) for _ in range(2)]
            for tc in range(16):
                i = tc % 2
                k.dma("sp", h0t[i], h0_scr[s, tc * 128:(tc + 1) * 128, :])
                for dh in range(2):
                    ps = bank[2 * i + dh]
                    for dcp in range(8):
                        k.mm(ps, mergedT[:, dcp, tc * 128:(tc + 1) * 128], wo[:, dcp, dh * 512:(dh + 1) * 512], dcp == 0, dcp == 7)
                    k.stt("dve", yb_[i][:, dh * 512:(dh + 1) * 512], h0t[i][:, dh * 512:(dh + 1) * 512], ALPHA, ps, ALU.mult, ALU.add)
                layer_norm(yb_[i], lnp, h1t[i], st[i], mv[i], sc[i])
                k.dma("sp", h1_scr[s, tc * 128:(tc + 1) * 128, :], h1t[i])
                k.copy("act", h1b[:, s, tc, :], h1t[i])
                pt_ = pp[2 + i]
                for dc in range(8):
                    k.tr(pt_[:, dc * 128:(dc + 1) * 128], h1t[i][:, dc * 128:(dc + 1) * 128], ident_f)
                k.copy("act", h1T[i], pt_[:, :].rearrange("p (a b) -> p a b", a=8))
                ps_l = bank[4 * i + 0 if False else (2 * i)]
                for dc in range(8):
                    k.mm(ps_l[:, 0:NE], h1T[i][:, dc, :], wr[:, dc, :], dc == 0, dc == 7)
                k.act(ex[i], ps_l[:, 0:NE], AF.Exp)
                P.op("dve", (lambda o, a_: (lambda e: e.reduce_sum(out=o, in_=a_, axis=AX.X)))(sm[i][:, 0:1], ex[i]),
                     [ex[i]], [sm[i][:, 0:1]])
                k.recip(sm[i][:, 1:2], sm[i][:, 0:1])
                k.ts("dve", aff[:, s, tc, :], ex[i], sm[i][:, 1:2], None, ALU.mult)

        if debug and 4 not in stages:
            P.skip = False
            affv = aff.rearrange("p a b c -> p (a b c)")
            k.dma("sp", affv, x_d[0, 0:128, 0:512])
            k.act(affv, affv, AF.Sigmoid)
        P.skip = 5 not in stages
        k.release(base_mark)
        posm = k.alloc([16, 64], F32)
        Rm = k.alloc([SPC, 16, NE, 4], BF16)
        idx_all = k.alloc([NE, 4], F32)
        gate_all = k.alloc([NE, 4], F32)
        route_mark = k.mark()
        affT = k.alloc([S], F32)
        junk = k.alloc([S], F32)
        lo_ = k.alloc([2], F32)
        mid = k.alloc([2], F32)
        cnt = k.alloc([2], F32)
        inc = k.alloc([2], F32)
        mask_b = k.alloc([16, 64], BF16)
        mask_f = k.alloc([16, 64], F32)
        rc0 = k.alloc([16, NE, 2], BF16)
        Lb = k.alloc([128], F32)
        taub = k.alloc([64], F32)
        k.dma("sp", rc0, rc0_d)
        k.memset("dve", affT[0:64, :], 0.0)
        for s in range(SPC):
            for tcg in range(4):
                ps = bank[(s * 4 + tcg) % 4]
                for l in range(4):
                    tc = tcg * 4 + l
                    k.tr(ps[0:16, l * 128:(l + 1) * 128], aff[:, s, tc, :], ident_f)
                k.copy("act", affT[32 * s:32 * s + 16, tcg * 512:(tcg + 1) * 512], ps[0:16, :])
        k.memset("dve", lo_[0:64, 0:1], 0.0)
        for itb in range(32):
            h = 2.0 ** (-(itb + 1))
            k.ts("dve", mid[0:64, 0:1], lo_[0:64, 0:1], h, None, ALU.add)
            k.ts("dve", junk[0:64, :], affT[0:64, :], mid[0:64, 0:1], 0.0, ALU.is_ge, ALU.add, accum=cnt[0:64, 0:1])
            k.ts("dve", inc[0:64, 0:1], cnt[0:64, 0:1], float(CAP) - 0.5, h, ALU.is_ge, ALU.mult)
            k.tt("dve", lo_[0:64, 0:1], lo_[0:64, 0:1], inc[0:64, 0:1], ALU.add)
        k.memset("pool", Lb[0:64, :], 1.0)
        k.ts("dve", Lb[0:64, :], Lb[0:64, :], lo_[0:64, 0:1], None, ALU.mult)
        k.mm(bank[0][:, 0:64], Lb[0:64, :], ident_f[0:64, 0:64], True, True)
        k.copy("act", taub, bank[0][:, 0:64])
        k.memset("pool", mask_f, 0.0)
        for s in range(SPC):
            k.tt("dve", mask_f[:, :, 32 * s:32 * s + 16], aff[:, s, :, :],
                 taub[:, 32 * s:32 * s + 16].unsqueeze(1).to_broadcast([128, 16, 16]), ALU.is_ge)
        k.copy("act", mask_b, mask_f)
        pq = pp[1]
        for tc in range(16):
            for t2 in range(tc):
                k.mm(pq[:, tc * 64:tc * 64 + 64], ones_bf, mask_b[:, t2, 0:64], t2 == 0, False)
            k.mm(pq[:, tc * 64:tc * 64 + 64], tri_bf, mask_b[:, tc, 0:64], tc == 0, True)
        pq3 = pq[:, :].rearrange("p (a b) -> p a b", a=16)
        k.memset("pool", posm, -1.0)
        k.stt("dve", posm[:, :, 0:64], pq3[:, :, 0:64], 1.0, mask_f[:, :, 0:64], ALU.add, ALU.mult)
        k.ts("dve", posm[:, :, 0:64], posm[:, :, 0:64], -1.0, None, ALU.add)
        for s in range(SPC):
            k.copy("dve", Rm[:, s, :, :, 0:2], rc0)
            k.copy("act", Rm[:, s, :, :, 2], aff[:, s, :, :])
            k.tt("dve", Rm[:, s, :, :, 3], aff[:, s, :, :], Rm[:, s, :, :, 2], ALU.subtract)
        if debug:
            k.dma("sp", aff_dbg, aff)

        P.skip = 6 not in stages
        k.release(route_mark)
        Pr = [k.alloc([16, CAP], BF16) for _ in range(3)]
        XeT = k.alloc([8, 512], BF16)
        HT = k.alloc([16, 512], BF16)
        wring = [k.alloc([16 * 512], BF16) for _ in range(3)]
        ye = [k.alloc([4, D], BF16) for _ in range(2)]
        sg = [k.alloc([512], F32) for _ in range(2)]
        infoS = k.alloc([4, 4], F32)
        wcnt = 0
        pcnt = 0
        hcnt = 0
        for e in range(NE):
            Ps = []
            for s in range(SPC):
                Pt = Pr[pcnt % 3]
                pcnt += 1
                Ps.append(Pt)
                col = 32 * s + e
                for tc in range(16):
                    k.ts("dve" if tc % 2 == 0 else "pool", Pt[:, tc, :], iota256, posm[:, tc, col:col + 1], None, ALU.is_equal)
                for cc in range(2):
                    kk = s * 2 + cc
                    for tc in range(16):
                        k.mm(bank[7][:, kk * 4:(kk + 1) * 4], Pt[:, tc, cc * 128:(cc + 1) * 128], Rm[:, s, tc, e, :], tc == 0, tc == 15)
            k.copy("act", infoS, bank[7][:, 0:16].rearrange("p (a b) -> p a b", a=4))
            k.tt("dve", gate_all[:, e, :], infoS[:, :, 2], infoS[:, :, 3], ALU.add)
            k.stt("dve", idx_all[:, e, :], infoS[:, :, 0], 128.0, infoS[:, :, 1], ALU.mult, ALU.add)
            for dc in range(8):
                ps_x = bank[dc % 2]
                for s in range(SPC):
                    for tc in range(16):
                        k.mm(ps_x[:, s * CAP:(s + 1) * CAP], h1b[:, s, tc, dc * 128:(dc + 1) * 128], Ps[s][:, tc, :], tc == 0, tc == 15)
                k.copy("act" if dc % 2 == 0 else "dve", XeT[:, dc, :], ps_x)
            for fq in range(4):
                wt = wring[wcnt % 3]
                wcnt += 1
                wg_s = wt[:, 0:4096].rearrange("p (a b) -> p a b", a=8)
                wu_s = wt[:, 4096:8192].rearrange("p (a b) -> p a b", a=8)
                fsl = slice(fq * 512, (fq + 1) * 512)
                k.dma("pool", wg_s, wg_d[e].rearrange("(c p) f -> p c f", p=128)[:, :, fsl])
                k.dma("pool", wu_s, wu_d[e].rearrange("(c p) f -> p c f", p=128)[:, :, fsl])
                for fl in range(4):
                    fc = fq * 4 + fl
                    j = hcnt % 2
                    hcnt += 1
                    ps_g, ps_u = bank[2 + 2 * j], bank[3 + 2 * j]
                    for dc in range(8):
                        k.mm(ps_g, wg_s[:, dc, fl * 128:(fl + 1) * 128], XeT[:, dc, :], dc == 0, dc == 7)
                    for dc in range(8):
                        k.mm(ps_u, wu_s[:, dc, fl * 128:(fl + 1) * 128], XeT[:, dc, :], dc == 0, dc == 7)
                    k.act(sg[j], ps_g, AF.Silu)
                    k.tt("dve", HT[:, fc, :], sg[j], ps_u, ALU.mult)
            yt = ye[e % 2]
            for dh in range(2):
                wt = wring[wcnt % 3]
                wcnt += 1
                wd_s = wt[:, :].rearrange("p (a b) -> p a b", a=16)
                wd_v = wd_d[e].rearrange("(c p) d -> p c d", p=128)
                k.dma("pool", wd_s[:, 0:8, :], wd_v[:, 0:8, dh * 512:(dh + 1) * 512])
                k.dma("pool", wd_s[:, 8:16, :], wd_v[:, 8:16, dh * 512:(dh + 1) * 512])
                for kk in range(4):
                    ps_y = bank[kk % 2]
                    for fc in range(16):
                        k.mm(ps_y, HT[:, fc, kk * 128:(kk + 1) * 128], wd_s[:, fc, :], fc == 0, fc == 15)
                    k.act(yt[:, kk, dh * 512:(dh + 1) * 512], ps_y, AF.Identity, scale=gate_all[:, e, kk:kk + 1])
            k.dma("sp", ye_scr[e].rearrange("s c p d -> p (s c) d"), yt)
        if debug:
            k.dma("sp", idx_dbg, idx_all)
            k.dma("sp", gate_dbg, gate_all)

        P.skip = 7 not in stages
        fin_mark = k.mark()
        for s in range(SPC):
            k.release(h1b_off)
            yes = k.alloc([NE, 2, D], BF16)
            k.off = route_mark
            for e in range(NE):
                k.dma("sp", yes[:, e, :, :], ye_scr[e, s].rearrange("c p d -> p c d"))
            PT = k.alloc([32, 512], BF16)
            iota = k.alloc([S], F32)
            k.dma("sp", iota, iota2048_d)
            lnp = k.alloc([2, D], F32)
            k.dma("sp", lnp, lnp_d[2])
            h1r = [k.alloc([D], F32) for _ in range(2)]
            yb_ = [k.alloc([D], F32) for _ in range(2)]
            ot = [k.alloc([D], F32) for _ in range(2)]
            st = [k.alloc([2, 6], F32) for _ in range(2)]
            mv = [k.alloc([2], F32) for _ in range(2)]
            sc = [k.alloc([2], F32) for _ in range(2)]
            for tq in range(4):
                for e in range(NE):
                    for cc in range(2):
                        kk = e * 2 + cc
                        k.ts("dve" if kk % 2 == 0 else "pool", PT[:, kk, :], iota[:, tq * 512:(tq + 1) * 512],
                             idx_all[:, e, s * 2 + cc:s * 2 + cc + 1], None, ALU.is_equal)
                for tcl in range(4):
                    tc = tq * 4 + tcl
                    i = tc % 2
                    k.dma("sp", h1r[i], h1_scr[s, tc * 128:(tc + 1) * 128, :])
                    for dh in range(2):
                        ps = bank[2 * i + dh]
                        for kk in range(32):
                            k.mm(ps, PT[:, kk, tcl * 128:(tcl + 1) * 128], yes[:, kk // 2, kk % 2, dh * 512:(dh + 1) * 512], kk == 0, kk == 31)
                        k.stt("dve", yb_[i][:, dh * 512:(dh + 1) * 512], h1r[i][:, dh * 512:(dh + 1) * 512], ALPHA, ps, ALU.mult, ALU.add)
                    layer_norm(yb_[i], lnp, ot[i], st[i], mv[i], sc[i])
                    k.dma("sp", out_d[s, tc * 128:(tc + 1) * 128, :], ot[i])
        P.skip = False
        P.emit()
    return nc


_CACHE = {}


def kernel(**inputs):
    inp = {k_: np.asarray(v) for k_, v in inputs.items()}
    hc = _host_consts()
    hw = _host_weights(inp)
    nc = build(False)
    x = inp["x"]
    in_maps = []
    for c in range(NCORE):
        m = {"x": np.ascontiguousarray(x[c * SPC:(c + 1) * SPC])}
        m.update(hw)
        m.update(hc)
        in_maps.append(m)
    res = run_bass_kernel_spmd(nc, in_maps, core_ids=list(range(NCORE)))
    out = np.concatenate([r["out"] for r in res.results], axis=0)
    return out.astype(np.float32)
```

```python
import numpy as np
import ml_dtypes
from contextlib import ExitStack
import concourse.bass as bass
import concourse.mybir as mybir
from concourse.bass_utils import run_bass_kernel_spmd

F32 = mybir.dt.float32
BF16 = mybir.dt.bfloat16
ALU = mybir.AluOpType
AF = mybir.ActivationFunctionType
AX = mybir.AxisListType

S = 2048
D = 1024
NCORE = 8
SPC = 2
NE = 16
CAP = 256
FF = 2048
ALPHA = float(2.0 ** 0.25)
LN_EPS = 1e-5
QK_EPS = 1e-6
HA = [0, 4, 1, 5, 2, 6, 3, 7]
DIL = (1, 4, 16)


def _isz(dt):
    return 4 if dt == F32 else 2


def _box(ap):
    a = ap.ap
    off = ap.offset
    isz = _isz(ap.dtype)
    sp = str(ap.space)
    if sp in ("SB", "PSUM"):
        pstep, pcnt = a[0]
        p0 = ap.base_partition()
        f0 = off - p0 * pstep
        ext = 0
        for st, ct in a[1:]:
            ext += abs(st) * (ct - 1)
        return (ap.name, p0, p0 + pcnt, f0 * isz, (f0 + ext + 1) * isz)
    ext = 0
    for st, ct in a:
        ext += abs(st) * (ct - 1)
    return (ap.name, 0, 1, off * isz, (off + ext + 1) * isz)


def _ovl(b1, b2):
    return b1[1] < b2[2] and b2[1] < b1[2] and b1[3] < b2[4] and b2[3] < b1[4]


def _cov(b1, b2):
    return b1[1] <= b2[1] and b1[2] >= b2[2] and b1[3] <= b2[3] and b1[4] >= b2[4]


class Op:
    __slots__ = ("eng", "fn", "deps", "signal", "sigval", "is_dma", "dsem", "dval", "idx", "prev_dma")

    def __init__(self, eng, fn):
        self.eng = eng
        self.fn = fn
        self.deps = []
        self.signal = False
        self.sigval = None
        self.is_dma = False
        self.dsem = None
        self.dval = None
        self.idx = None
        self.prev_dma = None


class Prog:
    ENGS = ("pe", "act", "dve", "pool", "sp")
    NDSEM = 8

    def __init__(self, nc):
        self.nc = nc
        self.ops = {e: [] for e in self.ENGS}
        self.acc = {}
        self.dma_count = {e: 0 for e in self.ENGS}
        self.dma_last = {}
        self.dma_slot_count = {}
        self.skip = False

    def _deps_for(self, op, reads, writes):
        deps = []
        rb_ = [_box(ap) for ap in reads]
        wb_ = [_box(ap) for ap in writes]
        for b in rb_:
            t = self.acc.get(b[0])
            if t is None:
                t = self.acc[b[0]] = {"w": [], "r": {}}
            for wb, wop in t["w"]:
                if _ovl(wb, b):
                    deps.append(wop)
        for b in wb_:
            t = self.acc.get(b[0])
            if t is None:
                t = self.acc[b[0]] = {"w": [], "r": {}}
            for wb, wop in t["w"]:
                if _ovl(wb, b):
                    deps.append(wop)
            for k, rop in t["r"].items():
                if _ovl(k[1], b):
                    deps.append(rop)
        ekey = ("dma", id(op)) if op.is_dma else op.eng
        for b in rb_:
            self.acc[b[0]]["r"][(ekey, b)] = op
        for b in wb_:
            t = self.acc[b[0]]
            t["w"] = [(wb, wop) for wb, wop in t["w"] if not _cov(b, wb)]
            t["w"].append((b, op))
            t["r"] = {k: v for k, v in t["r"].items() if not _cov(b, k[1])}
        best = {}
        out = []
        seen = set()
        for d in deps:
            if d is op:
                continue
            if d.is_dma:
                if id(d) not in seen:
                    seen.add(id(d))
                    out.append(d)
            else:
                if d.eng == "pe" and op.eng == "pe" and not op.is_dma:
                    continue
                cur = best.get(d.eng)
                if cur is None or d.idx > cur.idx:
                    best[d.eng] = d
        out.extend(best.values())
        op.deps = out
        for d in out:
            d.signal = True

    def op(self, eng, fn, reads=(), writes=()):
        if self.skip:
            return None
        o = Op(eng, fn)
        o.idx = len(self.ops[eng])
        self._deps_for(o, reads, writes)
        self.ops[eng].append(o)
        return o

    def dma(self, q, out, in_):
        if self.skip:
            return None
        o = Op(q, lambda e: e.dma_start(out=out, in_=in_))
        o.is_dma = True
        o.idx = len(self.ops[q])
        slot = self.dma_count[q] % self.NDSEM
        self.dma_count[q] += 1
        o.dsem = (q, slot)
        c = self.dma_slot_count.get((q, slot), 0) + 1
        self.dma_slot_count[(q, slot)] = c
        o.dval = 16 * c
        o.prev_dma = self.dma_last.get((q, slot))
        self.dma_last[(q, slot)] = o
        self._deps_for(o, [in_], [out])
        self.ops[q].append(o)
        return o

    def emit(self):
        nc = self.nc
        with ExitStack() as es:
            esem = {e: es.enter_context(nc.semaphore("s_" + e)) for e in self.ENGS}
            dsem = {}
            for q in self.ENGS:
                for s in range(min(self.NDSEM, self.dma_count[q])):
                    dsem[(q, s)] = es.enter_context(nc.semaphore("d_%s_%d" % (q, s)))
            for e in self.ENGS:
                c = 0
                for o in self.ops[e]:
                    if o.signal and not o.is_dma:
                        c += 1
                        o.sigval = c
            block = es.enter_context(nc.Block())

            def run(ename, eng):
                have = {}

                def wait(key, sem, val):
                    if have.get(key, 0) >= val:
                        return
                    eng.wait_ge(sem, val)
                    have[key] = val

                for o in self.ops[ename]:
                    for d in o.deps:
                        if d.is_dma:
                            wait(d.dsem, dsem[d.dsem], d.dval)
                        else:
                            wait(d.eng, esem[d.eng], d.sigval)
                    if o.is_dma:
                        if o.prev_dma is not None:
                            wait(o.dsem, dsem[o.dsem], o.prev_dma.dval)
                        o.fn(eng).then_inc(dsem[o.dsem], 16)
                    else:
                        ins = o.fn(eng)
                        if o.signal:
                            ins.then_inc(esem[ename], 1)
                if ename == "sp":
                    for key, o in self.dma_last.items():
                        wait(key, dsem[key], o.dval)

            @block.tensor
            def _(e):
                run("pe", e)

            @block.scalar
            def _(e):
                run("act", e)

            @block.vector
            def _(e):
                run("dve", e)

            @block.gpsimd
            def _(e):
                run("pool", e)

            @block.sync
            def _(e):
                run("sp", e)


def _aps(*xs):
    return [x for x in xs if not isinstance(x, (int, float)) and x is not None]


class K:
    def __init__(self, nc, P, big, total_bytes):
        self.nc = nc
        self.P = P
        self.big = big
        self.total = total_bytes
        self.off = 0

    def alloc(self, shape, dt):
        n = 1
        for s_ in shape:
            n *= s_
        nb = n * _isz(dt)
        nb_al = (nb + 63) // 64 * 64
        o = self.off
        self.off += nb_al
        assert self.off <= self.total, ("SBUF overflow", self.off, self.total)
        v = self.big[:, o // 2:(o + nb) // 2]
        if dt == F32:
            v = v.bitcast(F32)
        if len(shape) == 1:
            return v
        names = "abcdef"[:len(shape)]
        kw = {names[i]: shape[i] for i in range(len(shape) - 1)}
        return v.rearrange("p (" + " ".join(names) + ") -> p " + " ".join(names), **kw)

    def mark(self):
        return self.off

    def release(self, m):
        self.off = m

    def mm(self, out, lhsT, rhs, start=True, stop=True):
        self.P.op("pe", lambda e: e.matmul(out, lhsT=lhsT, rhs=rhs, start=start, stop=stop), [lhsT, rhs], [out])

    def tr(self, out, in_, ident):
        self.P.op("pe", lambda e: e.transpose(out, in_, ident), [in_, ident], [out])

    def act(self, out, in_, func, bias=0.0, scale=1.0):
        self.P.op("act", lambda e: e.activation(out=out, in_=in_, func=func, bias=bias, scale=scale),
                  _aps(in_, bias, scale), [out])

    def copy(self, eng, out, in_):
        if eng == "act":
            self.P.op("act", lambda e: e.copy(out=out, in_=in_), [in_], [out])
        else:
            self.P.op(eng, lambda e: e.tensor_copy(out=out, in_=in_), [in_], [out])

    def tt(self, eng, out, a, b, op):
        self.P.op(eng, lambda e: e.tensor_tensor(out=out, in0=a, in1=b, op=op), [a, b], [out])

    def ts(self, eng, out, a, s1, s2, op0, op1=None, accum=None):
        if op1 is None:
            self.P.op(eng, lambda e: e.tensor_scalar(out=out, in0=a, scalar1=s1, scalar2=None, op0=op0),
                      _aps(a, s1), [out])
        elif accum is None:
            self.P.op(eng, lambda e: e.tensor_scalar(out=out, in0=a, scalar1=s1, scalar2=s2, op0=op0, op1=op1),
                      _aps(a, s1, s2), [out])
        else:
            self.P.op(eng, lambda e: e.tensor_scalar(out=out, in0=a, scalar1=s1, scalar2=s2, op0=op0, op1=op1,
                                                     accum_out=accum), _aps(a, s1, s2), [out, accum])

    def stt(self, eng, out, in0, scalar, in1, op0, op1):
        self.P.op(eng, lambda e: e.scalar_tensor_tensor(out=out, in0=in0, scalar=scalar, in1=in1, op0=op0, op1=op1),
                  _aps(in0, scalar, in1), [out])

    def recip(self, out, in_):
        self.P.op("dve", lambda e: e.reciprocal(out=out, in_=in_), [in_], [out])

    def memset(self, eng, out, val):
        self.P.op(eng, lambda e: e.memset(out, val), [], [out])

    def dma(self, q, out, in_):
        self.P.dma(q, out, in_)


def _partnerA(d):
    return d + 16 if (d % 32) < 16 else d - 16


def _partnerB(d):
    if d < 8:
        return d + 8
    if d < 16:
        return d - 8
    return d


def _host_consts():
    f32 = np.float32
    t = np.arange(S)
    invA = (f32(10000.0) ** (-(np.arange(0, 32, 2, dtype=f32)) / f32(32))).astype(f32)
    ang_row = ((t // 64).astype(f32)[:, None] * invA[None, :]).astype(f32)
    ang_col = ((t % 64).astype(f32)[:, None] * invA[None, :]).astype(f32)
    invT = (f32(500000.0) ** (-(np.arange(0, 16, 2, dtype=f32)) / f32(16))).astype(f32)
    ang_t = (t.astype(f32)[:, None] * invT[None, :]).astype(f32)
    tabA = np.zeros((128, 2, S), f32)
    for p in range(128):
        dh = p % 64
        j = dh % 16
        x1 = (dh % 32) < 16
        ang = (ang_row if dh < 32 else ang_col)[:, j].astype(np.float64)
        tabA[p, 0] = np.cos(ang)
        tabA[p, 1] = -np.sin(ang) if x1 else np.sin(ang)
    tabB = np.zeros((3, 128, 2, S), f32)
    for g, r in enumerate(DIL):
        n = S // r
        tt_ = np.arange(S)
        perm = (tt_ % n) * r + (tt_ // n)
        for p in range(128):
            dh = p % 64
            if dh < 16:
                j = dh % 8
                ang = ang_t[perm, j].astype(np.float64)
                tabB[g, p, 0] = np.cos(ang)
                tabB[g, p, 1] = -np.sin(ang) if dh < 8 else np.sin(ang)
            else:
                tabB[g, p, 0] = 1.0
    c = {}
    c["tabA"] = tabA
    c["tabB"] = tabB
    c["ident_bf"] = np.eye(128, dtype=f32).astype(ml_dtypes.bfloat16)
    c["ident_f"] = np.eye(128, dtype=f32)
    k = np.arange(128)
    c["tri_bf"] = (k[:, None] < k[None, :]).astype(f32).astype(ml_dtypes.bfloat16)
    c["ones_bf"] = np.ones((128, 128), f32).astype(ml_dtypes.bfloat16)
    c["blk_f"] = ((k[:, None] // 64) == (k[None, :] // 64)).astype(f32)
    c["iota256"] = np.broadcast_to(np.arange(256, dtype=f32)[None, :], (128, 256)).copy()
    c["iota2048"] = np.broadcast_to(np.arange(2048, dtype=f32)[None, :], (128, 2048)).copy()
    q = np.arange(128)
    band = np.zeros((128, 3, 128), f32)
    band[:, 0, :] = ((k[:, None] - q[None, :]) >= 64)
    band[:, 1, :] = (np.abs(k[:, None] - q[None, :]) <= 64)
    band[:, 2, :] = ((q[None, :] - k[:, None]) >= 64)
    c["band"] = band.reshape(128, 384).astype(ml_dtypes.bfloat16)
    rc0 = np.zeros((128, 16, 16, 2), f32)
    rc0[:, :, :, 0] = np.arange(16, dtype=f32)[None, :, None]
    rc0[:, :, :, 1] = np.arange(128, dtype=f32)[:, None, None]
    c["rc0"] = rc0.astype(ml_dtypes.bfloat16)
    return c


def _host_weights(inp):
    w_in = inp["w_in"][0]
    o_qA, o_kA, o_vA, o_qB, o_kB, o_vB, o_gA = 0, 512, 640, 768, 1536, 2304, 3072
    cols = []
    for h in HA:
        cols += [o_qA + h * 64 + d for d in range(64)]
    for kv in range(2):
        cols += [o_kA + kv * 64 + d for d in range(64)]
    for h in HA:
        cols += [o_qA + h * 64 + _partnerA(d) for d in range(64)]
    for kv in range(2):
        cols += [o_kA + kv * 64 + _partnerA(d) for d in range(64)]
    cols += [o_vA + i for i in range(128)]
    wA = np.ascontiguousarray(w_in[:, cols])
    wB = np.zeros((6, D, 640), np.float32)
    for c in range(2):
        for g in range(3):
            hs = [4 * g + 2 * c, 4 * g + 2 * c + 1]
            cc = []
            for h in hs:
                cc += [o_qB + h * 64 + d for d in range(64)]
            for h in hs:
                cc += [o_kB + h * 64 + d for d in range(64)]
            for h in hs:
                cc += [o_qB + h * 64 + _partnerB(d) for d in range(64)]
            for h in hs:
                cc += [o_kB + h * 64 + _partnerB(d) for d in range(64)]
            for h in hs:
                cc += [o_vB + h * 64 + d for d in range(64)]
            wB[c * 3 + g] = w_in[:, cc]
    wG = np.ascontiguousarray(w_in[:, o_gA:o_gA + 2048])
    qn = inp["qn_g"][0]
    kn = inp["kn_g"][0]
    gvec = np.zeros((128, 4), np.float32)
    for p in range(128):
        d = p % 64
        gvec[p, 0] = qn[d]
        gvec[p, 1] = qn[_partnerA(d)]
        gvec[p, 2] = kn[d]
        gvec[p, 3] = kn[_partnerA(d)]
    bg = np.ascontiguousarray(inp["b_gate"][0].reshape(16, 128).T)
    rows = []
    for h in HA:
        rows += [h * 64 + d for d in range(64)]
    wa = np.ascontiguousarray(inp["w_branch_a"][0][rows, :])
    lnp = np.stack([inp["ln0_g"], inp["ln0_b"], inp["ln1_g"][0], inp["ln1_b"][0], inp["ln2_g"][0], inp["ln2_b"][0]])
    lnp = np.ascontiguousarray(np.broadcast_to(lnp.reshape(3, 1, 2, D), (3, 128, 2, D))).astype(np.float32)
    return dict(wA=wA, wB=wB, wG=wG, gvec=gvec, bgate=bg, wa=wa, wb=np.ascontiguousarray(inp["w_branch_b"][0]),
                wo=np.ascontiguousarray(inp["w_out"][0]), lnp=lnp, wr=np.ascontiguousarray(inp["w_router"][0]),
                wg_e=inp["w_gate_e"][0], wu_e=inp["w_up_e"][0], wd_e=inp["w_down_e"][0])


def build(debug=False, stage=7, only=None):
    stages = set(range(1, stage + 1)) if only is None else set(only)
    nc = bass.Bass("TRN2", target_bir_lowering=False)

    def din(name, shape, dt=F32):
        return nc.dram_tensor(name, list(shape), dt, kind="ExternalInput").ap()

    x_d = din("x", [SPC, S, D])
    wA_d = din("wA", [D, 1408])
    wB_d = din("wB", [6, D, 640])
    wG_d = din("wG", [D, 2048])
    gvec_d = din("gvec", [128, 4])
    bgate_d = din("bgate", [128, 16])
    wa_d = din("wa", [512, D])
    wb_d = din("wb", [256, D])
    wo_d = din("wo", [D, D])
    lnp_d = din("lnp", [3, 128, 2, D])
    wr_d = din("wr", [D, NE])
    wg_d = din("wg_e", [NE, D, FF])
    wu_d = din("wu_e", [NE, D, FF])
    wd_d = din("wd_e", [NE, FF, D])
    tabA_d = din("tabA", [128, 2, S])
    tabB_d = din("tabB", [3, 128, 2, S])
    identbf_d = din("ident_bf", [128, 128], BF16)
    identf_d = din("ident_f", [128, 128])
    tri_d = din("tri_bf", [128, 128], BF16)
    ones_d = din("ones_bf", [128, 128], BF16)
    blk_d = din("blk_f", [128, 128])
    iota256_d = din("iota256", [128, 256])
    iota2048_d = din("iota2048", [128, 2048])
    band_d = din("band", [128, 384], BF16)
    rc0_d = din("rc0", [128, 16, 16, 2], BF16)
    out_d = nc.dram_tensor("out", [SPC, S, D], F32, kind="ExternalOutput").ap()
    skind = "ExternalOutput" if debug else "Internal"
    h0_scr = nc.dram_tensor("h0_scr", [SPC, S, D], F32, kind=skind).ap()
    h1_scr = nc.dram_tensor("h1_scr", [SPC, S, D], F32, kind=skind).ap()
    ye_scr = nc.dram_tensor("ye_scr", [NE, SPC, 2, 128, D], BF16, kind="Internal").ap()
    if debug:
        aff_dbg = nc.dram_tensor("aff_dbg", [128, SPC, 16, NE], F32, kind="ExternalOutput").ap()
        idx_dbg = nc.dram_tensor("idx_dbg", [128, NE, 4], F32, kind="ExternalOutput").ap()
        gate_dbg = nc.dram_tensor("gate_dbg", [128, NE, 4], F32, kind="ExternalOutput").ap()

    TOTAL = 207 * 1024
    with ExitStack() as es:
        big = es.enter_context(nc.sbuf_tensor("big", [128, TOTAL // 2], BF16))
        pp = [es.enter_context(nc.psum_tensor("pp%d" % i, [128, 1024], F32)) for i in range(4)]
        bank = []
        for i in range(4):
            bank.append(pp[i][:, 0:512])
            bank.append(pp[i][:, 512:1024])
        P = Prog(nc)
        k = K(nc, P, big, TOTAL)

        ident_bf = k.alloc([128], BF16)
        ident_f = k.alloc([128], F32)
        tri_bf = k.alloc([128], BF16)
        ones_bf = k.alloc([128], BF16)
        blk_f = k.alloc([128], F32)
        iota256 = k.alloc([256], F32)
        band = k.alloc([384], BF16)
        gvec = k.alloc([4], F32)
        bgate = k.alloc([16], F32)
        wr = k.alloc([8, NE], F32)
        for dst, src in ((ident_bf, identbf_d), (ident_f, identf_d), (tri_bf, tri_d), (ones_bf, ones_d),
                         (blk_f, blk_d), (iota256, iota256_d), (band, band_d), (gvec, gvec_d), (bgate, bgate_d)):
            k.dma("sp", dst, src)
        k.dma("sp", wr, wr_d.rearrange("(c p) e -> p c e", p=128))
        h1b_off = k.mark()
        h1b = k.alloc([SPC, 16, D], BF16)
        aff = k.alloc([SPC, 16, NE], F32)
        base_mark = k.mark()

        def layer_norm(src, lnp, dst, st, mv, sc):
            for j in range(2):
                P.op("dve", (lambda o, i: (lambda e: e.bn_stats(out=o, in_=i)))(st[:, j, :], src[:, j * 512:(j + 1) * 512]),
                     [src[:, j * 512:(j + 1) * 512]], [st[:, j, :]])
            P.op("dve", lambda e: e.bn_aggr(out=mv, in_=st.rearrange("p a b -> p (a b)")), [st], [mv])
            k.act(sc[:, 0:1], mv[:, 1:2], AF.Sqrt, bias=LN_EPS, scale=1.0)
            k.recip(sc[:, 0:1], sc[:, 0:1])
            k.stt("dve", sc[:, 1:2], mv[:, 0:1], -1.0, sc[:, 0:1], ALU.mult, ALU.mult)
            k.act(dst, src, AF.Identity, bias=sc[:, 1:2], scale=sc[:, 0:1])
            k.tt("dve", dst, dst, lnp[:, 0, :], ALU.mult)
            k.tt("dve", dst, dst, lnp[:, 1, :], ALU.add)

        for s in range(SPC):
            k.release(base_mark)
            h0T = k.alloc([8, S], BF16)
            attnAT = k.alloc([4, S], BF16)
            obT = k.alloc([2, S], BF16)
            mixer_mark = k.mark()

            P.skip = 1 not in stages
            lnp = k.alloc([2, D], F32)
            k.dma("sp", lnp, lnp_d[0])
            xb = [k.alloc([D], F32) for _ in range(2)]
            hb = [k.alloc([D], F32) for _ in range(2)]
            hbb = [k.alloc([D], BF16) for _ in range(2)]
            st = [k.alloc([2, 6], F32) for _ in range(2)]
            mv = [k.alloc([2], F32) for _ in range(2)]
            sc = [k.alloc([2], F32) for _ in range(2)]
            k.dma("sp", xb[0], x_d[s, 0:128, :])
            for tc in range(16):
                i = tc % 2
                if tc + 1 < 16:
                    k.dma("sp", xb[1 - i], x_d[s, (tc + 1) * 128:(tc + 2) * 128, :])
                layer_norm(xb[i], lnp, hb[i], st[i], mv[i], sc[i])
                k.dma("sp", h0_scr[s, tc * 128:(tc + 1) * 128, :], hb[i])
                k.copy("act", hbb[i], hb[i])
                pb = pp[tc % 2][:, 0:512].bitcast(BF16)
                for dc in range(8):
                    k.tr(pb[:, dc * 128:(dc + 1) * 128], hbb[i][:, dc * 128:(dc + 1) * 128], ident_bf)
                k.copy("dve" if tc % 2 == 0 else "act", h0T[:, :, tc * 128:(tc + 1) * 128],
                       pb.rearrange("p (a b) -> p a b", a=8))
            k.release(mixer_mark)

            P.skip = 2 not in stages
            wA = k.alloc([8, 1408], BF16)
            wA_v = wA_d.rearrange("(c p) n -> p c n", p=128)
            k.dma("pool", wA[:, :, 0:704], wA_v[:, :, 0:704])
            k.dma("pool", wA[:, :, 704:1408], wA_v[:, :, 704:1408])
            qAT = k.alloc([4, S], BF16)
            kAT = k.alloc([S], BF16)
            VL = k.alloc([16, 128], BF16)
            VU = k.alloc([16, 128], BF16)
            k.memset("pool", VL[:, :, 64:128], 1.0)
            k.memset("pool", VU[:, :, 0:64], 1.0)
            tab = [k.alloc([2, 512], F32) for _ in range(2)]
            sq = [k.alloc([512], F32) for _ in range(2)]
            rs = [k.alloc([512], F32) for _ in range(2)]
            ta = [k.alloc([512], F32) for _ in range(2)]
            tb_ = [k.alloc([512], F32) for _ in range(2)]
            it = 0
            for tb in range(4):
                tsl = slice(tb * 512, (tb + 1) * 512)
                tbuf = tab[tb % 2]
                k.dma("sp", tbuf, tabA_d[:, :, tsl])
                for ch in range(5):
                    j = it % 2
                    it += 1
                    ps_m, ps_p, ps_ss = bank[2 * j], bank[2 * j + 1], bank[4 + j]
                    for dc in range(8):
                        k.mm(ps_m, wA[:, dc, ch * 128:(ch + 1) * 128], h0T[:, dc, tsl], dc == 0, dc == 7)
                    for dc in range(8):
                        k.mm(ps_p, wA[:, dc, 640 + ch * 128:640 + (ch + 1) * 128], h0T[:, dc, tsl], dc == 0, dc == 7)
                    k.act(sq[j], ps_m, AF.Square)
                    k.mm(ps_ss, blk_f, sq[j], True, True)
                    k.act(rs[j], ps_ss, AF.Sqrt, bias=QK_EPS, scale=1.0 / 64)
                    k.recip(rs[j], rs[j])
                    gi = 0 if ch < 4 else 2
                    k.stt("dve", ta[j], ps_m, gvec[:, gi:gi + 1], tbuf[:, 0, :], ALU.mult, ALU.mult)
                    k.stt("dve", tb_[j], ps_p, gvec[:, gi + 1:gi + 2], tbuf[:, 1, :], ALU.mult, ALU.mult)
                    k.tt("dve", ta[j], ta[j], tb_[j], ALU.add)
                    dst = qAT[:, ch, tsl] if ch < 4 else kAT[:, tsl]
                    k.tt("dve", dst, ta[j], rs[j], ALU.mult)
                for tcl in range(4):
                    tc = tb * 4 + tcl
                    ps_v = bank[6 + tcl % 2]
                    for dc in range(8):
                        k.mm(ps_v[:, 0:128], h0T[:, dc, tc * 128:(tc + 1) * 128], wA[:, dc, 1280:1408], dc == 0, dc == 7)
                    k.copy("act", VL[:, tc, 0:64], ps_v[:, 0:64])
                    k.copy("act", VU[:, tc, 64:128], ps_v[:, 64:128])
            pt = [k.alloc([512], BF16) for _ in range(3)]
            rc = [k.alloc([512], F32)] * 2
            steps = [(c, qb, hf, kc) for c in range(4) for qb in range(4) for hf in range(2) for kc in range(16)]

            def a_score(i):
                c, qb, hf, kc = steps[i]
                rows = slice(64 * hf, 64 * hf + 64)
                k.mm(bank[i % 3], kAT[rows, kc * 128:(kc + 1) * 128], qAT[rows, c, qb * 512:(qb + 1) * 512], True, True)

            a_score(0)
            for i in range(len(steps)):
                c, qb, hf, kc = steps[i]
                if i + 1 < len(steps):
                    a_score(i + 1)
                k.act(pt[i % 3], bank[i % 3], AF.Exp, scale=0.125)
                g_ = i // 16
                ps_o = bank[4 + g_ % 4]
                k.mm(ps_o, (VL if hf == 0 else VU)[:, kc, :], pt[i % 3], kc == 0, kc == 15)
                if kc == 15:
                    qsl = slice(qb * 512, (qb + 1) * 512)
                    r_ = rc[g_ % 2]
                    if hf == 0:
                        k.recip(r_[0:64, :], ps_o[64:128, :])
                        k.tt("dve", attnAT[0:64, c, qsl], ps_o[0:64, :], r_[0:64, :], ALU.mult)
                    else:
                        k.recip(r_[64:128, :], ps_o[0:64, :])
                        k.tt("dve", attnAT[64:128, c, qsl], ps_o[64:128, :], r_[64:128, :], ALU.mult)
            k.release(mixer_mark)

            P.skip = 3 not in stages
            accB = k.alloc([2, S], F32)
            wBs = k.alloc([8, 640], BF16)
            qBT = k.alloc([S], BF16)
            kBT = k.alloc([S], BF16)
            VBL = k.alloc([16, 128], BF16)
            VBU = k.alloc([16, 128], BF16)
            k.memset("pool", VBL[:, :, 64:128], 1.0)
            k.memset("pool", VBU[:, :, 0:64], 1.0)
            tab = [k.alloc([2, 512], F32) for _ in range(2)]
            ta = [k.alloc([512], F32) for _ in range(2)]
            tb_ = [k.alloc([512], F32) for _ in range(2)]
            eb = [k.alloc([384], BF16) for _ in range(3)]
            ptb = [k.alloc([384], BF16) for _ in range(3)]
            rcb = [k.alloc([512], F32) for _ in range(2)]
            it = 0
            for c in range(2):
                for g in range(3):
                    r = DIL[g]
                    n = S // r
                    ncc = n // 128
                    k.dma("pool", wBs, wB_d[c * 3 + g].rearrange("(c p) n -> p c n", p=128))

                    def hv(dc, p0, cnt):
                        v = h0T[:, dc, :].rearrange("p (m r) -> p r m", r=r)
                        if cnt <= n:
                            rho, m0 = p0 // n, p0 % n
                            return v[:, rho, m0:m0 + cnt]
                        rho0 = p0 // n
                        return v[:, rho0:rho0 + cnt // n, :]

                    for jb in range(4):
                        tsl = slice(jb * 512, (jb + 1) * 512)
                        tbuf = tab[it % 2]
                        k.dma("sp", tbuf, tabB_d[g][:, :, tsl])
                        for ch in range(2):
                            j = it % 2
                            it += 1
                            ps_m, ps_p = bank[2 * j], bank[2 * j + 1]
                            if n >= 512:
                                om, op_ = ps_m, ps_p
                            else:
                                om = ps_m.rearrange("p (a b) -> p a b", a=512 // n)
                                op_ = ps_p.rearrange("p (a b) -> p a b", a=512 // n)
                            for dc in range(8):
                                k.mm(om, wBs[:, dc, ch * 128:(ch + 1) * 128], hv(dc, jb * 512, 512), dc == 0, dc == 7)
                            for dc in range(8):
                                k.mm(op_, wBs[:, dc, 256 + ch * 128:256 + (ch + 1) * 128], hv(dc, jb * 512, 512), dc == 0, dc == 7)
                            k.tt("dve", ta[j], ps_m, tbuf[:, 0, :], ALU.mult)
                            k.tt("dve", tb_[j], ps_p, tbuf[:, 1, :], ALU.mult)
                            k.tt("dve", (qBT if ch == 0 else kBT)[:, tsl], ta[j], tb_[j], ALU.add)
                        for l in range(4):
                            kc = jb * 4 + l
                            ps_v = bank[6 + l % 2]
                            for dc in range(8):
                                k.mm(ps_v[:, 0:128], hv(dc, kc * 128, 128), wBs[:, dc, 512:640], dc == 0, dc == 7)
                            k.copy("act", VBL[:, kc, 0:64], ps_v[:, 0:64])
                            k.copy("act", VBU[:, kc, 64:128], ps_v[:, 64:128])
                    bsteps = [(hf, rho, a) for hf in range(2) for rho in range(r) for a in range(ncc)]

                    def b_score(i):
                        hf, rho, a = bsteps[i]
                        rows = slice(64 * hf, 64 * hf + 64)
                        qc = rho * ncc + a
                        ps_s = bank[i % 3]
                        for jj in range(3):
                            jn = a - 1 + jj
                            if 0 <= jn < ncc:
                                kc = rho * ncc + jn
                                k.mm(ps_s[:, jj * 128:(jj + 1) * 128], kBT[rows, kc * 128:(kc + 1) * 128],
                                     qBT[rows, qc * 128:(qc + 1) * 128], True, True)

                    b_score(0)
                    for i in range(len(bsteps)):
                        hf, rho, a = bsteps[i]
                        if i + 1 < len(bsteps):
                            b_score(i + 1)
                        jjs = [jj for jj in range(3) if 0 <= a - 1 + jj < ncc]
                        lo, hi = jjs[0] * 128, (jjs[-1] + 1) * 128
                        k.act(eb[i % 3][:, lo:hi], bank[i % 3][:, lo:hi], AF.Exp, scale=0.125)
                        k.tt("dve", ptb[i % 3][:, lo:hi], eb[i % 3][:, lo:hi], band[:, lo:hi], ALU.mult)
                        ps_o = bank[4 + i % 4]
                        for jj in jjs:
                            kc = rho * ncc + a - 1 + jj
                            k.mm(ps_o[:, 0:128], (VBL if hf == 0 else VBU)[:, kc, :], ptb[i % 3][:, jj * 128:(jj + 1) * 128],
                                 jj == jjs[0], jj == jjs[-1])
                        accv = accB[:, hf, :].rearrange("p (m r) -> p r m", r=r)[:, rho, a * 128:(a + 1) * 128]
                        if g == 0:
                            k.copy("dve", accv, ps_o[:, 0:128])
                        else:
                            k.tt("dve", accv, ps_o[:, 0:128], accv, ALU.add)
                for qb in range(4):
                    qsl = slice(qb * 512, (qb + 1) * 512)
                    r_ = rcb[qb % 2]
                    k.recip(r_[0:64, :], accB[64:128, 0, qsl])
                    k.tt("dve", obT[0:64, c, qsl], accB[0:64, 0, qsl], r_[0:64, :], ALU.mult)
                    k.recip(r_[64:128, :], accB[0:64, 1, qsl])
                    k.tt("dve", obT[64:128, c, qsl], accB[64:128, 1, qsl], r_[64:128, :], ALU.mult)
            k.release(mixer_mark)

            P.skip = 4 not in stages
            mergedT = k.alloc([8, S], BF16)
            p3_mark = k.mark()
            wgs = [k.alloc([8, 256], BF16) for _ in range(2)]
            was = [k.alloc([4, 128], BF16) for _ in range(2)]
            wbs = [k.alloc([2, 128], BF16) for _ in range(2)]
            gA = [k.alloc([512], F32) for _ in range(2)]
            gB = [k.alloc([512], F32) for _ in range(2)]
            m1 = [k.alloc([512], F32) for _ in range(2)]
            m2 = [k.alloc([512], F32) for _ in range(2)]
            wG_v = wG_d.rearrange("(c p) n -> p c n", p=128)
            wa_v = wa_d.rearrange("(c p) n -> p c n", p=128)
            wb_v = wb_d.rearrange("(c p) n -> p c n", p=128)
            it = 0
            for dcp in range(8):
                i = dcp % 2
                dsl = slice(dcp * 128, (dcp + 1) * 128)
                k.dma("pool", wgs[i][:, :, 0:128], wG_v[:, :, dsl])
                k.dma("pool", wgs[i][:, :, 128:256], wG_v[:, :, 1024 + dcp * 128:1024 + (dcp + 1) * 128])
                k.dma("pool", was[i], wa_v[:, :, dsl])
                k.dma("pool", wbs[i], wb_v[:, :, dsl])
                for tb in range(4):
                    tsl = slice(tb * 512, (tb + 1) * 512)
                    j = it % 2
                    it += 1
                    ps_ga, ps_gb, ps_ya, ps_yb = bank[4 * j], bank[4 * j + 1], bank[4 * j + 2], bank[4 * j + 3]
                    for dc in range(8):
                        k.mm(ps_ga, wgs[i][:, dc, 0:128], h0T[:, dc, tsl], dc == 0, dc == 7)
                    for dc in range(8):
                        k.mm(ps_gb, wgs[i][:, dc, 128:256], h0T[:, dc, tsl], dc == 0, dc == 7)
                    for c in range(4):
                        k.mm(ps_ya, was[i][:, c, :], attnAT[:, c, tsl], c == 0, c == 3)
                    for c in range(2):
                        k.mm(ps_yb, wbs[i][:, c, :], obT[:, c, tsl], c == 0, c == 1)
                    k.act(gA[j], ps_ga, AF.Sigmoid, bias=bgate[:, dcp:dcp + 1])
                    k.act(gB[j], ps_gb, AF.Sigmoid, bias=bgate[:, 8 + dcp:9 + dcp])
                    k.tt("dve", m1[j], gA[j], ps_ya, ALU.mult)
                    k.tt("dve", m2[j], gB[j], ps_yb, ALU.mult)
                    k.tt("dve", mergedT[:, dcp, tsl], m1[j], m2[j], ALU.add)
            k.release(p3_mark)

            wo = k.alloc([8, D], BF16)
            wo_v = wo_d.rearrange("(c p) n -> p c n", p=128)
            k.dma("pool", wo[:, :, 0:512], wo_v[:, :, 0:512])
            k.dma("pool", wo[:, :, 512:1024], wo_v[:, :, 512:1024])
            lnp = k.alloc([2, D], F32)
            k.dma("sp", lnp, lnp_d[1])
            h0t = [k.alloc([D], F32) for _ in range(2)]
            yb_ = [k.alloc([D], F32)] * 2
            h1t = [k.alloc([D], F32) for _ in range(2)]
            h1T = [k.alloc([8, 128], F32)] * 2
            st = [k.alloc([2, 6], F32) for _ in range(2)]
            mv = [k.alloc([2], F32) for _ in range(2)]
            sc = [k.alloc([2], F32) for _ in range(2)]
            ex = [k.alloc([NE], F32) for _ in range(2)]
            sm = [k.alloc([2], F32) for _ in range(2)]
            k.dma("sp", h0t[0], h0_scr[s, 0:128, :])
            for tc in range(16):
                i = tc % 2
                if tc + 1 < 16:
                    k.dma("sp", h0t[1 - i], h0_scr[s, (tc + 1) * 128:(tc + 2) * 128, :])
                for dh in range(2):
                    ps = bank[2 * i + dh]
                    for dcp in range(8):
                        k.mm(ps, mergedT[:, dcp, tc * 128:(tc + 1) * 128], wo[:, dcp, dh * 512:(dh + 1) * 512], dcp == 0, dcp == 7)
                    k.stt("dve", yb_[i][:, dh * 512:(dh + 1) * 512], h0t[i][:, dh * 512:(dh + 1) * 512], ALPHA, ps, ALU.mult, ALU.add)
                layer_norm(yb_[i], lnp, h1t[i], st[i], mv[i], sc[i])
                k.dma("sp", h1_scr[s, tc * 128:(tc + 1) * 128, :], h1t[i])
                k.copy("act", h1b[:, s, tc, :], h1t[i])
                pt_ = pp[2 + i]
                for dc in range(8):
                    k.tr(pt_[:, dc * 128:(dc + 1) * 128], h1t[i][:, dc * 128:(dc + 1) * 128], ident_f)
                k.copy("act", h1T[i], pt_[:, :].rearrange("p (a b) -> p a b", a=8))
                ps_l = bank[4 * i + 0 if False else (2 * i)]
                for dc in range(8):
                    k.mm(ps_l[:, 0:NE], h1T[i][:, dc, :], wr[:, dc, :], dc == 0, dc == 7)
                k.act(ex[i], ps_l[:, 0:NE], AF.Exp)
                P.op("dve", (lambda o, a_: (lambda e: e.reduce_sum(out=o, in_=a_, axis=AX.X)))(sm[i][:, 0:1], ex[i]),
                     [ex[i]], [sm[i][:, 0:1]])
                k.recip(sm[i][:, 1:2], sm[i][:, 0:1])
                k.ts("dve", aff[:, s, tc, :], ex[i], sm[i][:, 1:2], None, ALU.mult)

        if debug and 4 not in stages:
            P.skip = False
            affv = aff.rearrange("p a b c -> p (a b c)")
            k.dma("sp", affv, x_d[0, 0:128, 0:512])
            k.act(affv, affv, AF.Sigmoid)
        P.skip = 5 not in stages
        k.release(base_mark)
        posm = k.alloc([16, 64], F32)
        Rm = k.alloc([SPC, 16, NE, 4], BF16)
        idx_all = k.alloc([NE, 4], F32)
        gate_all = k.alloc([NE, 4], F32)
        route_mark = k.mark()
        affT = k.alloc([S], F32)
        junk = k.alloc([S], F32)
        lo_ = k.alloc([2], F32)
        mid = k.alloc([2], F32)
        cnt = k.alloc([2], F32)
        inc = k.alloc([2], F32)
        mask_b = k.alloc([16, 64], BF16)
        mask_f = k.alloc([16, 64], F32)
        rc0 = k.alloc([16, NE, 2], BF16)
        Lb = k.alloc([128], F32)
        taub = k.alloc([64], F32)
        k.dma("sp", rc0, rc0_d)
        k.memset("dve", affT[0:64, :], 0.0)
        for s in range(SPC):
            for tcg in range(4):
                ps = bank[(s * 4 + tcg) % 4]
                for l in range(4):
                    tc = tcg * 4 + l
                    k.tr(ps[0:16, l * 128:(l + 1) * 128], aff[:, s, tc, :], ident_f)
                k.copy("act", affT[32 * s:32 * s + 16, tcg * 512:(tcg + 1) * 512], ps[0:16, :])
        k.memset("dve", lo_[0:64, 0:1], 0.0)
        for itb in range(32):
            h = 2.0 ** (-(itb + 1))
            k.ts("dve", mid[0:64, 0:1], lo_[0:64, 0:1], h, None, ALU.add)
            k.ts("dve", junk[0:64, :], affT[0:64, :], mid[0:64, 0:1], 0.0, ALU.is_ge, ALU.add, accum=cnt[0:64, 0:1])
            k.ts("dve", inc[0:64, 0:1], cnt[0:64, 0:1], float(CAP) - 0.5, h, ALU.is_ge, ALU.mult)
            k.tt("dve", lo_[0:64, 0:1], lo_[0:64, 0:1], inc[0:64, 0:1], ALU.add)
        k.memset("pool", Lb[0:64, :], 1.0)
        k.ts("dve", Lb[0:64, :], Lb[0:64, :], lo_[0:64, 0:1], None, ALU.mult)
        k.mm(bank[0][:, 0:64], Lb[0:64, :], ident_f[0:64, 0:64], True, True)
        k.copy("act", taub, bank[0][:, 0:64])
        k.memset("pool", mask_f, 0.0)
        for s in range(SPC):
            k.tt("dve", mask_f[:, :, 32 * s:32 * s + 16], aff[:, s, :, :],
                 taub[:, 32 * s:32 * s + 16].unsqueeze(1).to_broadcast([128, 16, 16]), ALU.is_ge)
        k.copy("act", mask_b, mask_f)
        pq = pp[1]
        for tc in range(16):
            for t2 in range(tc):
                k.mm(pq[:, tc * 64:tc * 64 + 64], ones_bf, mask_b[:, t2, 0:64], t2 == 0, False)
            k.mm(pq[:, tc * 64:tc * 64 + 64], tri_bf, mask_b[:, tc, 0:64], tc == 0, True)
        pq3 = pq[:, :].rearrange("p (a b) -> p a b", a=16)
        k.memset("pool", posm, -1.0)
        k.stt("dve", posm[:, :, 0:64], pq3[:, :, 0:64], 1.0, mask_f[:, :, 0:64], ALU.add, ALU.mult)
        k.ts("dve", posm[:, :, 0:64], posm[:, :, 0:64], -1.0, None, ALU.add)
        for s in range(SPC):
            k.copy("dve", Rm[:, s, :, :, 0:2], rc0)
            k.copy("act", Rm[:, s, :, :, 2], aff[:, s, :, :])
            k.tt("dve", Rm[:, s, :, :, 3], aff[:, s, :, :], Rm[:, s, :, :, 2], ALU.subtract)
        if debug:
            k.dma("sp", aff_dbg, aff)

        P.skip = 6 not in stages
        k.release(route_mark)
        Pr = [k.alloc([16, CAP], BF16) for _ in range(3)]
        XeT = k.alloc([8, 512], BF16)
        HT = k.alloc([16, 512], BF16)
        wring = [k.alloc([16 * 512], BF16) for _ in range(3)]
        ye = [k.alloc([4, D], BF16) for _ in range(2)]
        sg = [k.alloc([512], F32) for _ in range(2)]
        infoS = k.alloc([4, 4], F32)
        wcnt = 0
        pcnt = 0
        hcnt = 0
        for e in range(NE):
            Ps = []
            for s in range(SPC):
                Pt = Pr[pcnt % 3]
                pcnt += 1
                Ps.append(Pt)
                col = 32 * s + e
                k.tt("dve", Pt, iota256.unsqueeze(1).to_broadcast([128, 16, CAP]),
                     posm[:, :, col:col + 1].to_broadcast([128, 16, CAP]), ALU.is_equal)
                for cc in range(2):
                    kk = s * 2 + cc
                    for tc in range(16):
                        k.mm(bank[7][:, kk * 4:(kk + 1) * 4], Pt[:, tc, cc * 128:(cc + 1) * 128], Rm[:, s, tc, e, :], tc == 0, tc == 15)
            k.copy("act", infoS, bank[7][:, 0:16].rearrange("p (a b) -> p a b", a=4))
            k.tt("dve", gate_all[:, e, :], infoS[:, :, 2], infoS[:, :, 3], ALU.add)
            k.stt("dve", idx_all[:, e, :], infoS[:, :, 0], 128.0, infoS[:, :, 1], ALU.mult, ALU.add)
            for dc in range(8):
                ps_x = bank[dc % 2]
                for s in range(SPC):
                    for tc in range(16):
                        k.mm(ps_x[:, s * CAP:(s + 1) * CAP], h1b[:, s, tc, dc * 128:(dc + 1) * 128], Ps[s][:, tc, :], tc == 0, tc == 15)
                k.copy("act" if dc % 2 == 0 else "dve", XeT[:, dc, :], ps_x)
            for fq in range(4):
                wt = wring[wcnt % 3]
                wcnt += 1
                wg_s = wt[:, 0:4096].rearrange("p (a b) -> p a b", a=8)
                wu_s = wt[:, 4096:8192].rearrange("p (a b) -> p a b", a=8)
                fsl = slice(fq * 512, (fq + 1) * 512)
                k.dma("pool", wg_s, wg_d[e].rearrange("(c p) f -> p c f", p=128)[:, :, fsl])
                k.dma("pool", wu_s, wu_d[e].rearrange("(c p) f -> p c f", p=128)[:, :, fsl])
                for fl in range(4):
                    fc = fq * 4 + fl
                    j = hcnt % 2
                    hcnt += 1
                    ps_g, ps_u = bank[2 + 2 * j], bank[3 + 2 * j]
                    for dc in range(8):
                        k.mm(ps_g, wg_s[:, dc, fl * 128:(fl + 1) * 128], XeT[:, dc, :], dc == 0, dc == 7)
                    for dc in range(8):
                        k.mm(ps_u, wu_s[:, dc, fl * 128:(fl + 1) * 128], XeT[:, dc, :], dc == 0, dc == 7)
                    k.act(sg[j], ps_g, AF.Silu)
                    k.tt("dve", HT[:, fc, :], sg[j], ps_u, ALU.mult)
            yt = ye[e % 2]
            for dh in range(2):
                wt = wring[wcnt % 3]
                wcnt += 1
                wd_s = wt[:, :].rearrange("p (a b) -> p a b", a=16)
                wd_v = wd_d[e].rearrange("(c p) d -> p c d", p=128)
                k.dma("pool", wd_s[:, 0:8, :], wd_v[:, 0:8, dh * 512:(dh + 1) * 512])
                k.dma("pool", wd_s[:, 8:16, :], wd_v[:, 8:16, dh * 512:(dh + 1) * 512])
                for kk in range(4):
                    ps_y = bank[kk % 2]
                    for fc in range(16):
                        k.mm(ps_y, HT[:, fc, kk * 128:(kk + 1) * 128], wd_s[:, fc, :], fc == 0, fc == 15)
                    k.act(yt[:, kk, dh * 512:(dh + 1) * 512], ps_y, AF.Identity, scale=gate_all[:, e, kk:kk + 1])
            k.dma("sp", ye_scr[e].rearrange("s c p d -> p (s c) d"), yt)
        if debug:
            k.dma("sp", idx_dbg, idx_all)
            k.dma("sp", gate_dbg, gate_all)

        P.skip = 7 not in stages
        fin_mark = k.mark()
        for s in range(SPC):
            k.release(h1b_off)
            yes = k.alloc([NE, 2, D], BF16)
            k.off = route_mark
            for e in range(NE):
                k.dma("sp", yes[:, e, :, :], ye_scr[e, s].rearrange("c p d -> p c d"))
            PT = k.alloc([32, 512], BF16)
            iota = k.alloc([S], F32)
            k.dma("sp", iota, iota2048_d)
            lnp = k.alloc([2, D], F32)
            k.dma("sp", lnp, lnp_d[2])
            h1r = [k.alloc([D], F32) for _ in range(2)]
            yb_ = [k.alloc([D], F32) for _ in range(2)]
            ot = [k.alloc([D], F32) for _ in range(2)]
            st = [k.alloc([2, 6], F32) for _ in range(2)]
            mv = [k.alloc([2], F32) for _ in range(2)]
            sc = [k.alloc([2], F32) for _ in range(2)]
            for tq in range(4):
                k.tt("dve", PT.rearrange("p (e c) d -> p e c d", c=2),
                     iota[:, tq * 512:(tq + 1) * 512].unsqueeze(1).unsqueeze(1).to_broadcast([128, NE, 2, 512]),
                     idx_all[:, :, s * 2:s * 2 + 2].unsqueeze(3).to_broadcast([128, NE, 2, 512]), ALU.is_equal)
                if tq == 0:
                    k.dma("sp", h1r[0], h1_scr[s, 0:128, :])
                for tcl in range(4):
                    tc = tq * 4 + tcl
                    i = tc % 2
                    if tc + 1 < 16:
                        k.dma("sp", h1r[1 - i], h1_scr[s, (tc + 1) * 128:(tc + 2) * 128, :])
                    for dh in range(2):
                        ps = bank[2 * i + dh]
                        for kk in range(32):
                            k.mm(ps, PT[:, kk, tcl * 128:(tcl + 1) * 128], yes[:, kk // 2, kk % 2, dh * 512:(dh + 1) * 512], kk == 0, kk == 31)
                        k.stt("dve", yb_[i][:, dh * 512:(dh + 1) * 512], h1r[i][:, dh * 512:(dh + 1) * 512], ALPHA, ps, ALU.mult, ALU.add)
                    layer_norm(yb_[i], lnp, ot[i], st[i], mv[i], sc[i])
                    k.dma("sp", out_d[s, tc * 128:(tc + 1) * 128, :], ot[i])
        P.skip = False
        P.emit()
    return nc


_CACHE = {}


def kernel(**inputs):
    inp = {k_: np.asarray(v) for k_, v in inputs.items()}
    hc = _host_consts()
    hw = _host_weights(inp)
    nc = build(False)
    x = inp["x"]
    in_maps = []
    for c in range(NCORE):
        m = {"x": np.ascontiguousarray(x[c * SPC:(c + 1) * SPC])}
        m.update(hw)
        m.update(hc)
        in_maps.append(m)
    res = run_bass_kernel_spmd(nc, in_maps, core_ids=list(range(NCORE)))
    out = np.concatenate([r["out"] for r in res.results], axis=0)
    return out.astype(np.float32)
```

```python
import numpy as np
import ml_dtypes
from contextlib import ExitStack
import concourse.bass as bass
import concourse.mybir as mybir
from concourse.bass_utils import run_bass_kernel_spmd

F32 = mybir.dt.float32
BF16 = mybir.dt.bfloat16
ALU = mybir.AluOpType
AF = mybir.ActivationFunctionType
AX = mybir.AxisListType

S = 2048
D = 1024
NCORE = 8
SPC = 2
NE = 16
CAP = 256
FF = 2048
ALPHA = float(2.0 ** 0.25)
LN_EPS = 1e-5
QK_EPS = 1e-6
HA = [0, 4, 1, 5, 2, 6, 3, 7]
DIL = (1, 4, 16)


def _isz(dt):
    return 4 if dt == F32 else 2


def _box(ap):
    a = ap.ap
    off = ap.offset
    isz = _isz(ap.dtype)
    sp = str(ap.space)
    if sp in ("SB", "PSUM"):
        pstep, pcnt = a[0]
        p0 = ap.base_partition()
        f0 = off - p0 * pstep
        ext = 0
        for st, ct in a[1:]:
            ext += abs(st) * (ct - 1)
        return (ap.name, p0, p0 + pcnt, f0 * isz, (f0 + ext + 1) * isz)
    ext = 0
    for st, ct in a:
        ext += abs(st) * (ct - 1)
    return (ap.name, 0, 1, off * isz, (off + ext + 1) * isz)


def _ovl(b1, b2):
    return b1[1] < b2[2] and b2[1] < b1[2] and b1[3] < b2[4] and b2[3] < b1[4]


def _cov(b1, b2):
    return b1[1] <= b2[1] and b1[2] >= b2[2] and b1[3] <= b2[3] and b1[4] >= b2[4]


class Op:
    __slots__ = ("eng", "fn", "deps", "signal", "sigval", "is_dma", "dsem", "dval", "idx", "prev_dma")

    def __init__(self, eng, fn):
        self.eng = eng
        self.fn = fn
        self.deps = []
        self.signal = False
        self.sigval = None
        self.is_dma = False
        self.dsem = None
        self.dval = None
        self.idx = None
        self.prev_dma = None


class Prog:
    ENGS = ("pe", "act", "dve", "pool", "sp")
    NDSEM = 8

    def __init__(self, nc):
        self.nc = nc
        self.ops = {e: [] for e in self.ENGS}
        self.acc = {}
        self.dma_count = {e: 0 for e in self.ENGS}
        self.dma_last = {}
        self.dma_slot_count = {}
        self.skip = False

    def _deps_for(self, op, reads, writes):
        deps = []
        rb_ = [_box(ap) for ap in reads]
        wb_ = [_box(ap) for ap in writes]
        for b in rb_:
            t = self.acc.get(b[0])
            if t is None:
                t = self.acc[b[0]] = {"w": [], "r": {}}
            for wb, wop in t["w"]:
                if _ovl(wb, b):
                    deps.append(wop)
        for b in wb_:
            t = self.acc.get(b[0])
            if t is None:
                t = self.acc[b[0]] = {"w": [], "r": {}}
            for wb, wop in t["w"]:
                if _ovl(wb, b):
                    deps.append(wop)
            for k, rop in t["r"].items():
                if _ovl(k[1], b):
                    deps.append(rop)
        ekey = ("dma", id(op)) if op.is_dma else op.eng
        for b in rb_:
            self.acc[b[0]]["r"][(ekey, b)] = op
        for b in wb_:
            t = self.acc[b[0]]
            t["w"] = [(wb, wop) for wb, wop in t["w"] if not _cov(b, wb)]
            t["w"].append((b, op))
            t["r"] = {k: v for k, v in t["r"].items() if not _cov(b, k[1])}
        best = {}
        out = []
        seen = set()
        for d in deps:
            if d is op:
                continue
            if d.is_dma:
                if id(d) not in seen:
                    seen.add(id(d))
                    out.append(d)
            else:
                if d.eng == "pe" and op.eng == "pe" and not op.is_dma:
                    continue
                cur = best.get(d.eng)
                if cur is None or d.idx > cur.idx:
                    best[d.eng] = d
        out.extend(best.values())
        op.deps = out
        for d in out:
            d.signal = True

    def op(self, eng, fn, reads=(), writes=()):
        if self.skip:
            return None
        o = Op(eng, fn)
        o.idx = len(self.ops[eng])
        self._deps_for(o, reads, writes)
        self.ops[eng].append(o)
        return o

    def dma(self, q, out, in_):
        if self.skip:
            return None
        o = Op(q, lambda e: e.dma_start(out=out, in_=in_))
        o.is_dma = True
        o.idx = len(self.ops[q])
        slot = self.dma_count[q] % self.NDSEM
        self.dma_count[q] += 1
        o.dsem = (q, slot)
        c = self.dma_slot_count.get((q, slot), 0) + 1
        self.dma_slot_count[(q, slot)] = c
        o.dval = 16 * c
        o.prev_dma = self.dma_last.get((q, slot))
        self.dma_last[(q, slot)] = o
        self._deps_for(o, [in_], [out])
        self.ops[q].append(o)
        return o

    def emit(self):
        nc = self.nc
        with ExitStack() as es:
            esem = {e: es.enter_context(nc.semaphore("s_" + e)) for e in self.ENGS}
            dsem = {}
            for q in self.ENGS:
                for s in range(min(self.NDSEM, self.dma_count[q])):
                    dsem[(q, s)] = es.enter_context(nc.semaphore("d_%s_%d" % (q, s)))
            for e in self.ENGS:
                c = 0
                for o in self.ops[e]:
                    if o.signal and not o.is_dma:
                        c += 1
                        o.sigval = c
            block = es.enter_context(nc.Block())

            def run(ename, eng):
                have = {}

                def wait(key, sem, val):
                    if have.get(key, 0) >= val:
                        return
                    eng.wait_ge(sem, val)
                    have[key] = val

                for o in self.ops[ename]:
                    for d in o.deps:
                        if d.is_dma:
                            wait(d.dsem, dsem[d.dsem], d.dval)
                        else:
                            wait(d.eng, esem[d.eng], d.sigval)
                    if o.is_dma:
                        if o.prev_dma is not None:
                            wait(o.dsem, dsem[o.dsem], o.prev_dma.dval)
                        o.fn(eng).then_inc(dsem[o.dsem], 16)
                    else:
                        ins = o.fn(eng)
                        if o.signal:
                            ins.then_inc(esem[ename], 1)
                if ename == "sp":
                    for key, o in self.dma_last.items():
                        wait(key, dsem[key], o.dval)

            @block.tensor
            def _(e):
                run("pe", e)

            @block.scalar
            def _(e):
                run("act", e)

            @block.vector
            def _(e):
                run("dve", e)

            @block.gpsimd
            def _(e):
                run("pool", e)

            @block.sync
            def _(e):
                run("sp", e)


def _aps(*xs):
    return [x for x in xs if not isinstance(x, (int, float)) and x is not None]


class K:
    def __init__(self, nc, P, big, total_bytes):
        self.nc = nc
        self.P = P
        self.big = big
        self.total = total_bytes
        self.off = 0

    def alloc(self, shape, dt):
        n = 1
        for s_ in shape:
            n *= s_
        nb = n * _isz(dt)
        nb_al = (nb + 63) // 64 * 64
        o = self.off
        self.off += nb_al
        assert self.off <= self.total, ("SBUF overflow", self.off, self.total)
        v = self.big[:, o // 2:(o + nb) // 2]
        if dt == F32:
            v = v.bitcast(F32)
        if len(shape) == 1:
            return v
        names = "abcdef"[:len(shape)]
        kw = {names[i]: shape[i] for i in range(len(shape) - 1)}
        return v.rearrange("p (" + " ".join(names) + ") -> p " + " ".join(names), **kw)

    def mark(self):
        return self.off

    def release(self, m):
        self.off = m

    def mm(self, out, lhsT, rhs, start=True, stop=True):
        self.P.op("pe", lambda e: e.matmul(out, lhsT=lhsT, rhs=rhs, start=start, stop=stop), [lhsT, rhs], [out])

    def tr(self, out, in_, ident):
        self.P.op("pe", lambda e: e.transpose(out, in_, ident), [in_, ident], [out])

    def act(self, out, in_, func, bias=0.0, scale=1.0):
        self.P.op("act", lambda e: e.activation(out=out, in_=in_, func=func, bias=bias, scale=scale),
                  _aps(in_, bias, scale), [out])

    def copy(self, eng, out, in_):
        if eng == "act":
            self.P.op("act", lambda e: e.copy(out=out, in_=in_), [in_], [out])
        else:
            self.P.op(eng, lambda e: e.tensor_copy(out=out, in_=in_), [in_], [out])

    def tt(self, eng, out, a, b, op):
        self.P.op(eng, lambda e: e.tensor_tensor(out=out, in0=a, in1=b, op=op), [a, b], [out])

    def ts(self, eng, out, a, s1, s2, op0, op1=None, accum=None):
        if op1 is None:
            self.P.op(eng, lambda e: e.tensor_scalar(out=out, in0=a, scalar1=s1, scalar2=None, op0=op0),
                      _aps(a, s1), [out])
        elif accum is None:
            self.P.op(eng, lambda e: e.tensor_scalar(out=out, in0=a, scalar1=s1, scalar2=s2, op0=op0, op1=op1),
                      _aps(a, s1, s2), [out])
        else:
            self.P.op(eng, lambda e: e.tensor_scalar(out=out, in0=a, scalar1=s1, scalar2=s2, op0=op0, op1=op1,
                                                     accum_out=accum), _aps(a, s1, s2), [out, accum])

    def stt(self, eng, out, in0, scalar, in1, op0, op1):
        self.P.op(eng, lambda e: e.scalar_tensor_tensor(out=out, in0=in0, scalar=scalar, in1=in1, op0=op0, op1=op1),
                  _aps(in0, scalar, in1), [out])

    def recip(self, out, in_):
        self.P.op("dve", lambda e: e.reciprocal(out=out, in_=in_), [in_], [out])

    def memset(self, eng, out, val):
        self.P.op(eng, lambda e: e.memset(out, val), [], [out])

    def dma(self, q, out, in_):
        self.P.dma(q, out, in_)


def _partnerA(d):
    return d + 16 if (d % 32) < 16 else d - 16


def _partnerB(d):
    if d < 8:
        return d + 8
    if d < 16:
        return d - 8
    return d


def _host_consts():
    f32 = np.float32
    t = np.arange(S)
    invA = (f32(10000.0) ** (-(np.arange(0, 32, 2, dtype=f32)) / f32(32))).astype(f32)
    ang_row = ((t // 64).astype(f32)[:, None] * invA[None, :]).astype(f32)
    ang_col = ((t % 64).astype(f32)[:, None] * invA[None, :]).astype(f32)
    invT = (f32(500000.0) ** (-(np.arange(0, 16, 2, dtype=f32)) / f32(16))).astype(f32)
    ang_t = (t.astype(f32)[:, None] * invT[None, :]).astype(f32)
    tabA = np.zeros((128, 2, S), f32)
    for p in range(128):
        dh = p % 64
        j = dh % 16
        x1 = (dh % 32) < 16
        ang = (ang_row if dh < 32 else ang_col)[:, j].astype(np.float64)
        tabA[p, 0] = np.cos(ang)
        tabA[p, 1] = -np.sin(ang) if x1 else np.sin(ang)
    tabB = np.zeros((3, 128, 2, S), f32)
    for g, r in enumerate(DIL):
        n = S // r
        tt_ = np.arange(S)
        perm = (tt_ % n) * r + (tt_ // n)
        for p in range(128):
            dh = p % 64
            if dh < 16:
                j = dh % 8
                ang = ang_t[perm, j].astype(np.float64)
                tabB[g, p, 0] = np.cos(ang)
                tabB[g, p, 1] = -np.sin(ang) if dh < 8 else np.sin(ang)
            else:
                tabB[g, p, 0] = 1.0
    c = {}
    c["tabA"] = tabA
    c["tabB"] = tabB
    c["ident_bf"] = np.eye(128, dtype=f32).astype(ml_dtypes.bfloat16)
    c["ident_f"] = np.eye(128, dtype=f32)
    k = np.arange(128)
    c["tri_bf"] = (k[:, None] < k[None, :]).astype(f32).astype(ml_dtypes.bfloat16)
    c["ones_bf"] = np.ones((128, 128), f32).astype(ml_dtypes.bfloat16)
    c["blk_f"] = ((k[:, None] // 64) == (k[None, :] // 64)).astype(f32)
    c["iota256"] = np.broadcast_to(np.arange(256, dtype=f32)[None, :], (128, 256)).copy()
    c["iota2048"] = np.broadcast_to(np.arange(2048, dtype=f32)[None, :], (128, 2048)).copy()
    q = np.arange(128)
    band = np.zeros((128, 3, 128), f32)
    band[:, 0, :] = ((k[:, None] - q[None, :]) >= 64)
    band[:, 1, :] = (np.abs(k[:, None] - q[None, :]) <= 64)
    band[:, 2, :] = ((q[None, :] - k[:, None]) >= 64)
    c["band"] = band.reshape(128, 384).astype(ml_dtypes.bfloat16)
    rc0 = np.zeros((128, 16, 16, 2), f32)
    rc0[:, :, :, 0] = np.arange(16, dtype=f32)[None, :, None]
    rc0[:, :, :, 1] = np.arange(128, dtype=f32)[:, None, None]
    c["rc0"] = rc0.astype(ml_dtypes.bfloat16)
    return c


def _host_weights(inp):
    w_in = inp["w_in"][0]
    o_qA, o_kA, o_vA, o_qB, o_kB, o_vB, o_gA = 0, 512, 640, 768, 1536, 2304, 3072
    cols = []
    for h in HA:
        cols += [o_qA + h * 64 + d for d in range(64)]
    for kv in range(2):
        cols += [o_kA + kv * 64 + d for d in range(64)]
    for h in HA:
        cols += [o_qA + h * 64 + _partnerA(d) for d in range(64)]
    for kv in range(2):
        cols += [o_kA + kv * 64 + _partnerA(d) for d in range(64)]
    cols += [o_vA + i for i in range(128)]
    wA = np.ascontiguousarray(w_in[:, cols])
    wB = np.zeros((6, D, 640), np.float32)
    for c in range(2):
        for g in range(3):
            hs = [4 * g + 2 * c, 4 * g + 2 * c + 1]
            cc = []
            for h in hs:
                cc += [o_qB + h * 64 + d for d in range(64)]
            for h in hs:
                cc += [o_kB + h * 64 + d for d in range(64)]
            for h in hs:
                cc += [o_qB + h * 64 + _partnerB(d) for d in range(64)]
            for h in hs:
                cc += [o_kB + h * 64 + _partnerB(d) for d in range(64)]
            for h in hs:
                cc += [o_vB + h * 64 + d for d in range(64)]
            wB[c * 3 + g] = w_in[:, cc]
    wG = np.ascontiguousarray(w_in[:, o_gA:o_gA + 2048])
    qn = inp["qn_g"][0]
    kn = inp["kn_g"][0]
    gvec = np.zeros((128, 4), np.float32)
    for p in range(128):
        d = p % 64
        gvec[p, 0] = qn[d]
        gvec[p, 1] = qn[_partnerA(d)]
        gvec[p, 2] = kn[d]
        gvec[p, 3] = kn[_partnerA(d)]
    bg = np.ascontiguousarray(inp["b_gate"][0].reshape(16, 128).T)
    rows = []
    for h in HA:
        rows += [h * 64 + d for d in range(64)]
    wa = np.ascontiguousarray(inp["w_branch_a"][0][rows, :])
    lnp = np.stack([inp["ln0_g"], inp["ln0_b"], inp["ln1_g"][0], inp["ln1_b"][0], inp["ln2_g"][0], inp["ln2_b"][0]])
    lnp = np.ascontiguousarray(np.broadcast_to(lnp.reshape(3, 1, 2, D), (3, 128, 2, D))).astype(np.float32)
    return dict(wA=wA, wB=wB, wG=wG, gvec=gvec, bgate=bg, wa=wa, wb=np.ascontiguousarray(inp["w_branch_b"][0]),
                wo=np.ascontiguousarray(inp["w_out"][0]), lnp=lnp, wr=np.ascontiguousarray(inp["w_router"][0]),
                wg_e=inp["w_gate_e"][0], wu_e=inp["w_up_e"][0], wd_e=inp["w_down_e"][0])


def build(debug=False, stage=7, only=None):
    stages = set(range(1, stage + 1)) if only is None else set(only)
    nc = bass.Bass("TRN2", target_bir_lowering=False)

    def din(name, shape, dt=F32):
        return nc.dram_tensor(name, list(shape), dt, kind="ExternalInput").ap()

    x_d = din("x", [SPC, S, D])
    wA_d = din("wA", [D, 1408])
    wB_d = din("wB", [6, D, 640])
    wG_d = din("wG", [D, 2048])
    gvec_d = din("gvec", [128, 4])
    bgate_d = din("bgate", [128, 16])
    wa_d = din("wa", [512, D])
    wb_d = din("wb", [256, D])
    wo_d = din("wo", [D, D])
    lnp_d = din("lnp", [3, 128, 2, D])
    wr_d = din("wr", [D, NE])
    wg_d = din("wg_e", [NE, D, FF])
    wu_d = din("wu_e", [NE, D, FF])
    wd_d = din("wd_e", [NE, FF, D])
    tabA_d = din("tabA", [128, 2, S])
    tabB_d = din("tabB", [3, 128, 2, S])
    identbf_d = din("ident_bf", [128, 128], BF16)
    identf_d = din("ident_f", [128, 128])
    tri_d = din("tri_bf", [128, 128], BF16)
    ones_d = din("ones_bf", [128, 128], BF16)
    blk_d = din("blk_f", [128, 128])
    iota256_d = din("iota256", [128, 256])
    iota2048_d = din("iota2048", [128, 2048])
    band_d = din("band", [128, 384], BF16)
    rc0_d = din("rc0", [128, 16, 16, 2], BF16)
    out_d = nc.dram_tensor("out", [SPC, S, D], F32, kind="ExternalOutput").ap()
    skind = "ExternalOutput" if debug else "Internal"
    h0_scr = nc.dram_tensor("h0_scr", [SPC, S, D], F32, kind=skind).ap()
    h1_scr = nc.dram_tensor("h1_scr", [SPC, S, D], F32, kind=skind).ap()
    ye_scr = nc.dram_tensor("ye_scr", [NE, SPC, 2, 128, D], BF16, kind="Internal").ap()
    if debug:
        aff_dbg = nc.dram_tensor("aff_dbg", [128, SPC, 16, NE], F32, kind="ExternalOutput").ap()
        idx_dbg = nc.dram_tensor("idx_dbg", [128, NE, 4], F32, kind="ExternalOutput").ap()
        gate_dbg = nc.dram_tensor("gate_dbg", [128, NE, 4], F32, kind="ExternalOutput").ap()

    TOTAL = 207 * 1024
    with ExitStack() as es:
        big = es.enter_context(nc.sbuf_tensor("big", [128, TOTAL // 2], BF16))
        pp = [es.enter_context(nc.psum_tensor("pp%d" % i, [128, 1024], F32)) for i in range(4)]
        bank = []
        for i in range(4):
            bank.append(pp[i][:, 0:512])
            bank.append(pp[i][:, 512:1024])
        P = Prog(nc)
        k = K(nc, P, big, TOTAL)

        ident_bf = k.alloc([128], BF16)
        ident_f = k.alloc([128], F32)
        tri_bf = k.alloc([128], BF16)
        ones_bf = k.alloc([128], BF16)
        blk_f = k.alloc([128], F32)
        iota256 = k.alloc([256], F32)
        band = k.alloc([384], BF16)
        gvec = k.alloc([4], F32)
        bgate = k.alloc([16], F32)
        wr = k.alloc([8, NE], F32)
        for dst, src in ((ident_bf, identbf_d), (ident_f, identf_d), (tri_bf, tri_d), (ones_bf, ones_d),
                         (blk_f, blk_d), (iota256, iota256_d), (band, band_d), (gvec, gvec_d), (bgate, bgate_d)):
            k.dma("sp", dst, src)
        k.dma("sp", wr, wr_d.rearrange("(c p) e -> p c e", p=128))
        h1b_off = k.mark()
        h1b = k.alloc([SPC, 16, D], BF16)
        aff = k.alloc([SPC, 16, NE], F32)
        base_mark = k.mark()

        def layer_norm(src, lnp, dst, st, mv, sc):
            for j in range(2):
                P.op("dve", (lambda o, i: (lambda e: e.bn_stats(out=o, in_=i)))(st[:, j, :], src[:, j * 512:(j + 1) * 512]),
                     [src[:, j * 512:(j + 1) * 512]], [st[:, j, :]])
            P.op("dve", lambda e: e.bn_aggr(out=mv, in_=st.rearrange("p a b -> p (a b)")), [st], [mv])
            k.act(sc[:, 0:1], mv[:, 1:2], AF.Sqrt, bias=LN_EPS, scale=1.0)
            k.recip(sc[:, 0:1], sc[:, 0:1])
            k.stt("dve", sc[:, 1:2], mv[:, 0:1], -1.0, sc[:, 0:1], ALU.mult, ALU.mult)
            k.act(dst, src, AF.Identity, bias=sc[:, 1:2], scale=sc[:, 0:1])
            k.tt("dve", dst, dst, lnp[:, 0, :], ALU.mult)
            k.tt("dve", dst, dst, lnp[:, 1, :], ALU.add)

        for s in range(SPC):
            k.release(base_mark)
            h0T = k.alloc([8, S], BF16)
            attnAT = k.alloc([4, S], BF16)
            obT = k.alloc([2, S], BF16)
            mixer_mark = k.mark()

            P.skip = 1 not in stages
            lnp = k.alloc([2, D], F32)
            k.dma("sp", lnp, lnp_d[0])
            xb = [k.alloc([D], F32) for _ in range(2)]
            hb = [k.alloc([D], F32) for _ in range(2)]
            hbb = [k.alloc([D], BF16) for _ in range(2)]
            st = [k.alloc([2, 6], F32) for _ in range(2)]
            mv = [k.alloc([2], F32) for _ in range(2)]
            sc = [k.alloc([2], F32) for _ in range(2)]
            k.dma("sp", xb[0], x_d[s, 0:128, :])
            for tc in range(16):
                i = tc % 2
                if tc + 1 < 16:
                    k.dma("sp", xb[1 - i], x_d[s, (tc + 1) * 128:(tc + 2) * 128, :])
                layer_norm(xb[i], lnp, hb[i], st[i], mv[i], sc[i])
                k.dma("sp", h0_scr[s, tc * 128:(tc + 1) * 128, :], hb[i])
                k.copy("act", hbb[i], hb[i])
                pb = pp[tc % 2][:, 0:512].bitcast(BF16)
                for dc in range(8):
                    k.tr(pb[:, dc * 128:(dc + 1) * 128], hbb[i][:, dc * 128:(dc + 1) * 128], ident_bf)
                k.copy("dve" if tc % 2 == 0 else "act", h0T[:, :, tc * 128:(tc + 1) * 128],
                       pb.rearrange("p (a b) -> p a b", a=8))
            k.release(mixer_mark)

            P.skip = 2 not in stages
            wA = k.alloc([8, 1408], BF16)
            wA_v = wA_d.rearrange("(c p) n -> p c n", p=128)
            k.dma("pool", wA[:, :, 0:704], wA_v[:, :, 0:704])
            k.dma("pool", wA[:, :, 704:1408], wA_v[:, :, 704:1408])
            qAT = k.alloc([4, S], BF16)
            kAT = k.alloc([S], BF16)
            VL = k.alloc([16, 128], BF16)
            VU = k.alloc([16, 128], BF16)
            k.memset("pool", VL[:, :, 64:128], 1.0)
            k.memset("pool", VU[:, :, 0:64], 1.0)
            tab = [k.alloc([2, 512], F32) for _ in range(2)]
            sq = [k.alloc([512], F32) for _ in range(2)]
            rs = [k.alloc([512], F32) for _ in range(2)]
            ta = [k.alloc([512], F32) for _ in range(2)]
            tb_ = [k.alloc([512], F32) for _ in range(2)]
            it = 0
            for tb in range(4):
                tsl = slice(tb * 512, (tb + 1) * 512)
                tbuf = tab[tb % 2]
                k.dma("sp", tbuf, tabA_d[:, :, tsl])
                for ch in range(5):
                    j = it % 2
                    it += 1
                    ps_m, ps_p, ps_ss = bank[2 * j], bank[2 * j + 1], bank[4 + j]
                    for dc in range(8):
                        k.mm(ps_m, wA[:, dc, ch * 128:(ch + 1) * 128], h0T[:, dc, tsl], dc == 0, dc == 7)
                    for dc in range(8):
                        k.mm(ps_p, wA[:, dc, 640 + ch * 128:640 + (ch + 1) * 128], h0T[:, dc, tsl], dc == 0, dc == 7)
                    k.act(sq[j], ps_m, AF.Square)
                    k.mm(ps_ss, blk_f, sq[j], True, True)
                    k.act(rs[j], ps_ss, AF.Sqrt, bias=QK_EPS, scale=1.0 / 64)
                    k.recip(rs[j], rs[j])
                    gi = 0 if ch < 4 else 2
                    k.stt("dve", ta[j], ps_m, gvec[:, gi:gi + 1], tbuf[:, 0, :], ALU.mult, ALU.mult)
                    k.stt("dve", tb_[j], ps_p, gvec[:, gi + 1:gi + 2], tbuf[:, 1, :], ALU.mult, ALU.mult)
                    k.tt("dve", ta[j], ta[j], tb_[j], ALU.add)
                    dst = qAT[:, ch, tsl] if ch < 4 else kAT[:, tsl]
                    k.tt("dve", dst, ta[j], rs[j], ALU.mult)
                for tcl in range(4):
                    tc = tb * 4 + tcl
                    ps_v = bank[6 + tcl % 2]
                    for dc in range(8):
                        k.mm(ps_v[:, 0:128], h0T[:, dc, tc * 128:(tc + 1) * 128], wA[:, dc, 1280:1408], dc == 0, dc == 7)
                    k.copy("act", VL[:, tc, 0:64], ps_v[:, 0:64])
                    k.copy("act", VU[:, tc, 64:128], ps_v[:, 64:128])
            pt = [k.alloc([512], BF16) for _ in range(3)]
            rc = [k.alloc([512], F32)] * 2
            steps = [(c, qb, hf, kc) for c in range(4) for qb in range(4) for hf in range(2) for kc in range(16)]

            def a_score(i):
                c, qb, hf, kc = steps[i]
                rows = slice(64 * hf, 64 * hf + 64)
                k.mm(bank[i % 3], kAT[rows, kc * 128:(kc + 1) * 128], qAT[rows, c, qb * 512:(qb + 1) * 512], True, True)

            a_score(0)
            for i in range(len(steps)):
                c, qb, hf, kc = steps[i]
                if i + 1 < len(steps):
                    a_score(i + 1)
                k.act(pt[i % 3], bank[i % 3], AF.Exp, scale=0.125)
                g_ = i // 16
                ps_o = bank[4 + g_ % 4]
                k.mm(ps_o, (VL if hf == 0 else VU)[:, kc, :], pt[i % 3], kc == 0, kc == 15)
                if kc == 15:
                    qsl = slice(qb * 512, (qb + 1) * 512)
                    r_ = rc[g_ % 2]
                    if hf == 0:
                        k.recip(r_[0:64, :], ps_o[64:128, :])
                        k.tt("dve", attnAT[0:64, c, qsl], ps_o[0:64, :], r_[0:64, :], ALU.mult)
                    else:
                        k.recip(r_[64:128, :], ps_o[0:64, :])
                        k.tt("dve", attnAT[64:128, c, qsl], ps_o[64:128, :], r_[64:128, :], ALU.mult)
            k.release(mixer_mark)

            P.skip = 3 not in stages
            accB = k.alloc([2, S], F32)
            wBs = k.alloc([8, 640], BF16)
            qBT = k.alloc([S], BF16)
            kBT = k.alloc([S], BF16)
            VBL = k.alloc([16, 128], BF16)
            VBU = k.alloc([16, 128], BF16)
            VTb = k.alloc([S], BF16)
            k.memset("pool", VBL[:, :, 64:128], 1.0)
            k.memset("pool", VBU[:, :, 0:64], 1.0)
            tab = [k.alloc([2, 512], F32) for _ in range(2)]
            ta = [k.alloc([512], F32) for _ in range(2)]
            tb_ = [k.alloc([512], F32) for _ in range(2)]
            eb = [k.alloc([384], BF16) for _ in range(3)]
            ptb = [k.alloc([384], BF16) for _ in range(3)]
            rcb = [k.alloc([512], F32) for _ in range(2)]
            it = 0
            for c in range(2):
                for g in range(3):
                    r = DIL[g]
                    n = S // r
                    ncc = n // 128
                    k.dma("pool", wBs, wB_d[c * 3 + g].rearrange("(c p) n -> p c n", p=128))

                    def hv(dc, p0, cnt):
                        v = h0T[:, dc, :].rearrange("p (m r) -> p r m", r=r)
                        rho, m0 = p0 // n, p0 % n
                        return v[:, rho, m0:m0 + cnt]

                    def pview(buf, jb):
                        if r == 1:
                            return buf[:, jb * 512:(jb + 1) * 512]
                        m0, m1 = jb * 512 // r, (jb + 1) * 512 // r
                        return buf.rearrange("p (r m) -> p m r", r=r)[:, m0:m1, :]

                    def nview(buf):
                        if r == 1:
                            return buf
                        return buf.rearrange("p (m r) -> p m r", r=r)

                    for jb in range(4):
                        tsl = slice(jb * 512, (jb + 1) * 512)
                        tbuf = tab[jb % 2]
                        k.dma("sp", tbuf, tabB_d[0][:, :, tsl])
                        for ch in range(2):
                            j = it % 2
                            it += 1
                            ps_m, ps_p = bank[2 * j], bank[2 * j + 1]
                            for dc in range(8):
                                k.mm(ps_m, wBs[:, dc, ch * 128:(ch + 1) * 128], h0T[:, dc, tsl], dc == 0, dc == 7)
                            for dc in range(8):
                                k.mm(ps_p, wBs[:, dc, 256 + ch * 128:256 + (ch + 1) * 128], h0T[:, dc, tsl], dc == 0, dc == 7)
                            k.tt("dve", ta[j], ps_m, tbuf[:, 0, :], ALU.mult)
                            k.tt("dve", tb_[j], ps_p, tbuf[:, 1, :], ALU.mult)
                            k.tt("dve", pview(qBT if ch == 0 else kBT, jb), nview(ta[j]), nview(tb_[j]), ALU.add)
                        for l in range(4):
                            kc = jb * 4 + l
                            ps_v = bank[6 + l % 2]
                            for dc in range(8):
                                k.mm(ps_v[:, 0:128], hv(dc, kc * 128, 128), wBs[:, dc, 512:640], dc == 0, dc == 7)
                            k.copy("act", VBL[:, kc, 0:64], ps_v[:, 0:64])
                            k.copy("act", VBU[:, kc, 64:128], ps_v[:, 64:128])
                    bsteps = [(hf, rho, a) for hf in range(2) for rho in range(r) for a in range(ncc)]

                    def b_score(i):
                        hf, rho, a = bsteps[i]
                        rows = slice(64 * hf, 64 * hf + 64)
                        qc = rho * ncc + a
                        ps_s = bank[i % 3]
                        for jj in range(3):
                            jn = a - 1 + jj
                            if 0 <= jn < ncc:
                                kc = rho * ncc + jn
                                k.mm(ps_s[:, jj * 128:(jj + 1) * 128], kBT[rows, kc * 128:(kc + 1) * 128],
                                     qBT[rows, qc * 128:(qc + 1) * 128], True, True)

                    b_score(0)
                    for i in range(len(bsteps)):
                        hf, rho, a = bsteps[i]
                        if i + 1 < len(bsteps):
                            b_score(i + 1)
                        jjs = [jj for jj in range(3) if 0 <= a - 1 + jj < ncc]
                        lo, hi = jjs[0] * 128, (jjs[-1] + 1) * 128
                        k.act(eb[i % 3][:, lo:hi], bank[i % 3][:, lo:hi], AF.Exp, scale=0.125)
                        k.tt("dve", ptb[i % 3][:, lo:hi], eb[i % 3][:, lo:hi], band[:, lo:hi], ALU.mult)
                        ps_o = bank[4 + i % 4]
                        for jj in jjs:
                            kc = rho * ncc + a - 1 + jj
                            k.mm(ps_o[:, 0:128], (VBL if hf == 0 else VBU)[:, kc, :], ptb[i % 3][:, jj * 128:(jj + 1) * 128],
                                 jj == jjs[0], jj == jjs[-1])
                        accv = accB[:, hf, :].rearrange("p (m r) -> p r m", r=r)[:, rho, a * 128:(a + 1) * 128]
                        if g == 0:
                            k.copy("dve", accv, ps_o[:, 0:128])
                        else:
                            k.tt("dve", accv, ps_o[:, 0:128], accv, ALU.add)
                for qb in range(4):
                    qsl = slice(qb * 512, (qb + 1) * 512)
                    r_ = rcb[qb % 2]
                    k.recip(r_[0:64, :], accB[64:128, 0, qsl])
                    k.tt("dve", obT[0:64, c, qsl], accB[0:64, 0, qsl], r_[0:64, :], ALU.mult)
                    k.recip(r_[64:128, :], accB[0:64, 1, qsl])
                    k.tt("dve", obT[64:128, c, qsl], accB[64:128, 1, qsl], r_[64:128, :], ALU.mult)
            k.release(mixer_mark)

            P.skip = 4 not in stages
            mergedT = k.alloc([8, S], BF16)
            p3_mark = k.mark()
            wgs = [k.alloc([8, 256], BF16) for _ in range(2)]
            was = [k.alloc([4, 128], BF16) for _ in range(2)]
            wbs = [k.alloc([2, 128], BF16) for _ in range(2)]
            gA = [k.alloc([512], F32) for _ in range(2)]
            gB = [k.alloc([512], F32) for _ in range(2)]
            m1 = [k.alloc([512], F32) for _ in range(2)]
            m2 = [k.alloc([512], F32) for _ in range(2)]
            wG_v = wG_d.rearrange("(c p) n -> p c n", p=128)
            wa_v = wa_d.rearrange("(c p) n -> p c n", p=128)
            wb_v = wb_d.rearrange("(c p) n -> p c n", p=128)
            it = 0
            for dcp in range(8):
                i = dcp % 2
                dsl = slice(dcp * 128, (dcp + 1) * 128)
                k.dma("pool", wgs[i][:, :, 0:128], wG_v[:, :, dsl])
                k.dma("pool", wgs[i][:, :, 128:256], wG_v[:, :, 1024 + dcp * 128:1024 + (dcp + 1) * 128])
                k.dma("pool", was[i], wa_v[:, :, dsl])
                k.dma("pool", wbs[i], wb_v[:, :, dsl])
                for tb in range(4):
                    tsl = slice(tb * 512, (tb + 1) * 512)
                    j = it % 2
                    it += 1
                    ps_ga, ps_gb, ps_ya, ps_yb = bank[4 * j], bank[4 * j + 1], bank[4 * j + 2], bank[4 * j + 3]
                    for dc in range(8):
                        k.mm(ps_ga, wgs[i][:, dc, 0:128], h0T[:, dc, tsl], dc == 0, dc == 7)
                    for dc in range(8):
                        k.mm(ps_gb, wgs[i][:, dc, 128:256], h0T[:, dc, tsl], dc == 0, dc == 7)
                    for c in range(4):
                        k.mm(ps_ya, was[i][:, c, :], attnAT[:, c, tsl], c == 0, c == 3)
                    for c in range(2):
                        k.mm(ps_yb, wbs[i][:, c, :], obT[:, c, tsl], c == 0, c == 1)
                    k.act(gA[j], ps_ga, AF.Sigmoid, bias=bgate[:, dcp:dcp + 1])
                    k.act(gB[j], ps_gb, AF.Sigmoid, bias=bgate[:, 8 + dcp:9 + dcp])
                    k.tt("dve", m1[j], gA[j], ps_ya, ALU.mult)
                    k.tt("dve", m2[j], gB[j], ps_yb, ALU.mult)
                    k.tt("dve", mergedT[:, dcp, tsl], m1[j], m2[j], ALU.add)
            k.release(p3_mark)

            wo = k.alloc([8, D], BF16)
            wo_v = wo_d.rearrange("(c p) n -> p c n", p=128)
            k.dma("pool", wo[:, :, 0:512], wo_v[:, :, 0:512])
            k.dma("pool", wo[:, :, 512:1024], wo_v[:, :, 512:1024])
            lnp = k.alloc([2, D], F32)
            k.dma("sp", lnp, lnp_d[1])
            h0t = [k.alloc([D], F32) for _ in range(2)]
            yb_ = [k.alloc([D], F32)] * 2
            h1t = [k.alloc([D], F32) for _ in range(2)]
            h1T = [k.alloc([8, 128], F32)] * 2
            st = [k.alloc([2, 6], F32) for _ in range(2)]
            mv = [k.alloc([2], F32) for _ in range(2)]
            sc = [k.alloc([2], F32) for _ in range(2)]
            ex = [k.alloc([NE], F32) for _ in range(2)]
            sm = [k.alloc([2], F32) for _ in range(2)]
            k.dma("sp", h0t[0], h0_scr[s, 0:128, :])
            for tc in range(16):
                i = tc % 2
                if tc + 1 < 16:
                    k.dma("sp", h0t[1 - i], h0_scr[s, (tc + 1) * 128:(tc + 2) * 128, :])
                for dh in range(2):
                    ps = bank[2 * i + dh]
                    for dcp in range(8):
                        k.mm(ps, mergedT[:, dcp, tc * 128:(tc + 1) * 128], wo[:, dcp, dh * 512:(dh + 1) * 512], dcp == 0, dcp == 7)
                    k.stt("dve", yb_[i][:, dh * 512:(dh + 1) * 512], h0t[i][:, dh * 512:(dh + 1) * 512], ALPHA, ps, ALU.mult, ALU.add)
                layer_norm(yb_[i], lnp, h1t[i], st[i], mv[i], sc[i])
                k.dma("sp", h1_scr[s, tc * 128:(tc + 1) * 128, :], h1t[i])
                k.copy("act", h1b[:, s, tc, :], h1t[i])
                pt_ = pp[2 + i]
                for dc in range(8):
                    k.tr(pt_[:, dc * 128:(dc + 1) * 128], h1t[i][:, dc * 128:(dc + 1) * 128], ident_f)
                k.copy("act", h1T[i], pt_[:, :].rearrange("p (a b) -> p a b", a=8))
                ps_l = bank[4 * i + 0 if False else (2 * i)]
                for dc in range(8):
                    k.mm(ps_l[:, 0:NE], h1T[i][:, dc, :], wr[:, dc, :], dc == 0, dc == 7)
                k.act(ex[i], ps_l[:, 0:NE], AF.Exp)
                P.op("dve", (lambda o, a_: (lambda e: e.reduce_sum(out=o, in_=a_, axis=AX.X)))(sm[i][:, 0:1], ex[i]),
                     [ex[i]], [sm[i][:, 0:1]])
                k.recip(sm[i][:, 1:2], sm[i][:, 0:1])
                k.ts("dve", aff[:, s, tc, :], ex[i], sm[i][:, 1:2], None, ALU.mult)

        if debug and 4 not in stages:
            P.skip = False
            affv = aff.rearrange("p a b c -> p (a b c)")
            k.dma("sp", affv, x_d[0, 0:128, 0:512])
            k.act(affv, affv, AF.Sigmoid)
        P.skip = 5 not in stages
        k.release(base_mark)
        posm = k.alloc([16, 64], F32)
        Rm = k.alloc([SPC, 16, NE, 4], BF16)
        idx_all = k.alloc([NE, 4], F32)
        gate_all = k.alloc([NE, 4], F32)
        route_mark = k.mark()
        affT = k.alloc([S], F32)
        junk = k.alloc([S], F32)
        lo_ = k.alloc([2], F32)
        mid = k.alloc([2], F32)
        cnt = k.alloc([2], F32)
        inc = k.alloc([2], F32)
        mask_b = k.alloc([16, 64], BF16)
        mask_f = k.alloc([16, 64], F32)
        rc0 = k.alloc([16, NE, 2], BF16)
        Lb = k.alloc([128], F32)
        taub = k.alloc([64], F32)
        k.dma("sp", rc0, rc0_d)
        k.memset("dve", affT[0:64, :], 0.0)
        for s in range(SPC):
            for tcg in range(4):
                ps = bank[(s * 4 + tcg) % 4]
                for l in range(4):
                    tc = tcg * 4 + l
                    k.tr(ps[0:16, l * 128:(l + 1) * 128], aff[:, s, tc, :], ident_f)
                k.copy("act", affT[32 * s:32 * s + 16, tcg * 512:(tcg + 1) * 512], ps[0:16, :])
        k.memset("dve", lo_[0:64, 0:1], 0.0)
        for itb in range(32):
            h = 2.0 ** (-(itb + 1))
            k.ts("dve", mid[0:64, 0:1], lo_[0:64, 0:1], h, None, ALU.add)
            k.ts("dve", junk[0:64, :], affT[0:64, :], mid[0:64, 0:1], 0.0, ALU.is_ge, ALU.add, accum=cnt[0:64, 0:1])
            k.ts("dve", inc[0:64, 0:1], cnt[0:64, 0:1], float(CAP) - 0.5, h, ALU.is_ge, ALU.mult)
            k.tt("dve", lo_[0:64, 0:1], lo_[0:64, 0:1], inc[0:64, 0:1], ALU.add)
        k.memset("pool", Lb[0:64, :], 1.0)
        k.ts("dve", Lb[0:64, :], Lb[0:64, :], lo_[0:64, 0:1], None, ALU.mult)
        k.mm(bank[0][:, 0:64], Lb[0:64, :], ident_f[0:64, 0:64], True, True)
        k.copy("act", taub, bank[0][:, 0:64])
        k.memset("pool", mask_f, 0.0)
        for s in range(SPC):
            k.tt("dve", mask_f[:, :, 32 * s:32 * s + 16], aff[:, s, :, :],
                 taub[:, 32 * s:32 * s + 16].unsqueeze(1).to_broadcast([128, 16, 16]), ALU.is_ge)
        k.copy("act", mask_b, mask_f)
        pq = pp[1]
        for tc in range(16):
            for t2 in range(tc):
                k.mm(pq[:, tc * 64:tc * 64 + 64], ones_bf, mask_b[:, t2, 0:64], t2 == 0, False)
            k.mm(pq[:, tc * 64:tc * 64 + 64], tri_bf, mask_b[:, tc, 0:64], tc == 0, True)
        pq3 = pq[:, :].rearrange("p (a b) -> p a b", a=16)
        k.memset("pool", posm, -1.0)
        k.stt("dve", posm[:, :, 0:64], pq3[:, :, 0:64], 1.0, mask_f[:, :, 0:64], ALU.add, ALU.mult)
        k.ts("dve", posm[:, :, 0:64], posm[:, :, 0:64], -1.0, None, ALU.add)
        for s in range(SPC):
            k.copy("dve", Rm[:, s, :, :, 0:2], rc0)
            k.copy("act", Rm[:, s, :, :, 2], aff[:, s, :, :])
            k.tt("dve", Rm[:, s, :, :, 3], aff[:, s, :, :], Rm[:, s, :, :, 2], ALU.subtract)
        if debug:
            k.dma("sp", aff_dbg, aff)

        P.skip = 6 not in stages
        k.release(route_mark)
        Pr = [k.alloc([16, CAP], BF16) for _ in range(3)]
        XeT = k.alloc([8, 512], BF16)
        HT = k.alloc([16, 512], BF16)
        wring = [k.alloc([16 * 512], BF16) for _ in range(3)]
        ye = [k.alloc([4, D], BF16) for _ in range(2)]
        sg = [k.alloc([512], F32) for _ in range(2)]
        infoS = k.alloc([4, 4], F32)
        wcnt = 0
        pcnt = 0
        hcnt = 0
        for e in range(NE):
            Ps = []
            for s in range(SPC):
                Pt = Pr[pcnt % 3]
                pcnt += 1
                Ps.append(Pt)
                col = 32 * s + e
                k.tt("dve", Pt, iota256.unsqueeze(1).to_broadcast([128, 16, CAP]),
                     posm[:, :, col:col + 1].to_broadcast([128, 16, CAP]), ALU.is_equal)
                for cc in range(2):
                    kk = s * 2 + cc
                    for tc in range(16):
                        k.mm(bank[7][:, kk * 4:(kk + 1) * 4], Pt[:, tc, cc * 128:(cc + 1) * 128], Rm[:, s, tc, e, :], tc == 0, tc == 15)
            k.copy("act", infoS, bank[7][:, 0:16].rearrange("p (a b) -> p a b", a=4))
            k.tt("dve", gate_all[:, e, :], infoS[:, :, 2], infoS[:, :, 3], ALU.add)
            k.stt("dve", idx_all[:, e, :], infoS[:, :, 0], 128.0, infoS[:, :, 1], ALU.mult, ALU.add)
            for dc in range(8):
                ps_x = bank[dc % 2]
                for s in range(SPC):
                    for tc in range(16):
                        k.mm(ps_x[:, s * CAP:(s + 1) * CAP], h1b[:, s, tc, dc * 128:(dc + 1) * 128], Ps[s][:, tc, :], tc == 0, tc == 15)
                k.copy("act" if dc % 2 == 0 else "dve", XeT[:, dc, :], ps_x)
            for fq in range(4):
                wt = wring[wcnt % 3]
                wcnt += 1
                wg_s = wt[:, 0:4096].rearrange("p (a b) -> p a b", a=8)
                wu_s = wt[:, 4096:8192].rearrange("p (a b) -> p a b", a=8)
                fsl = slice(fq * 512, (fq + 1) * 512)
                k.dma("pool", wg_s, wg_d[e].rearrange("(c p) f -> p c f", p=128)[:, :, fsl])
                k.dma("pool", wu_s, wu_d[e].rearrange("(c p) f -> p c f", p=128)[:, :, fsl])
                for fl in range(4):
                    fc = fq * 4 + fl
                    j = hcnt % 2
                    hcnt += 1
                    ps_g, ps_u = bank[2 + 2 * j], bank[3 + 2 * j]
                    for dc in range(8):
                        k.mm(ps_g, wg_s[:, dc, fl * 128:(fl + 1) * 128], XeT[:, dc, :], dc == 0, dc == 7)
                    for dc in range(8):
                        k.mm(ps_u, wu_s[:, dc, fl * 128:(fl + 1) * 128], XeT[:, dc, :], dc == 0, dc == 7)
                    k.act(sg[j], ps_g, AF.Silu)
                    k.tt("dve", HT[:, fc, :], sg[j], ps_u, ALU.mult)
            yt = ye[e % 2]
            for dh in range(2):
                wt = wring[wcnt % 3]
                wcnt += 1
                wd_s = wt[:, :].rearrange("p (a b) -> p a b", a=16)
                wd_v = wd_d[e].rearrange("(c p) d -> p c d", p=128)
                k.dma("pool", wd_s[:, 0:8, :], wd_v[:, 0:8, dh * 512:(dh + 1) * 512])
                k.dma("pool", wd_s[:, 8:16, :], wd_v[:, 8:16, dh * 512:(dh + 1) * 512])
                for kk in range(4):
                    ps_y = bank[kk % 2]
                    for fc in range(16):
                        k.mm(ps_y, HT[:, fc, kk * 128:(kk + 1) * 128], wd_s[:, fc, :], fc == 0, fc == 15)
                    k.act(yt[:, kk, dh * 512:(dh + 1) * 512], ps_y, AF.Identity, scale=gate_all[:, e, kk:kk + 1])
            k.dma("sp", ye_scr[e].rearrange("s c p d -> p (s c) d"), yt)
        if debug:
            k.dma("sp", idx_dbg, idx_all)
            k.dma("sp", gate_dbg, gate_all)

        P.skip = 7 not in stages
        fin_mark = k.mark()
        for s in range(SPC):
            k.release(h1b_off)
            yes = k.alloc([NE, 2, D], BF16)
            k.off = route_mark
            for e in range(NE):
                k.dma("sp", yes[:, e, :, :], ye_scr[e, s].rearrange("c p d -> p c d"))
            PT = k.alloc([32, 512], BF16)
            iota = k.alloc([S], F32)
            k.dma("sp", iota, iota2048_d)
            lnp = k.alloc([2, D], F32)
            k.dma("sp", lnp, lnp_d[2])
            h1r = [k.alloc([D], F32) for _ in range(2)]
            yb_ = [k.alloc([D], F32) for _ in range(2)]
            ot = [k.alloc([D], F32) for _ in range(2)]
            st = [k.alloc([2, 6], F32) for _ in range(2)]
            mv = [k.alloc([2], F32) for _ in range(2)]
            sc = [k.alloc([2], F32) for _ in range(2)]
            for tq in range(4):
                k.tt("dve", PT.rearrange("p (e c) d -> p e c d", c=2),
                     iota[:, tq * 512:(tq + 1) * 512].unsqueeze(1).unsqueeze(1).to_broadcast([128, NE, 2, 512]),
                     idx_all[:, :, s * 2:s * 2 + 2].unsqueeze(3).to_broadcast([128, NE, 2, 512]), ALU.is_equal)
                if tq == 0:
                    k.dma("sp", h1r[0], h1_scr[s, 0:128, :])
                for tcl in range(4):
                    tc = tq * 4 + tcl
                    i = tc % 2
                    if tc + 1 < 16:
                        k.dma("sp", h1r[1 - i], h1_scr[s, (tc + 1) * 128:(tc + 2) * 128, :])
                    for dh in range(2):
                        ps = bank[2 * i + dh]
                        for kk in range(32):
                            k.mm(ps, PT[:, kk, tcl * 128:(tcl + 1) * 128], yes[:, kk // 2, kk % 2, dh * 512:(dh + 1) * 512], kk == 0, kk == 31)
                        k.stt("dve", yb_[i][:, dh * 512:(dh + 1) * 512], h1r[i][:, dh * 512:(dh + 1) * 512], ALPHA, ps, ALU.mult, ALU.add)
                    layer_norm(yb_[i], lnp, ot[i], st[i], mv[i], sc[i])
                    k.dma("sp", out_d[s, tc * 128:(tc + 1) * 128, :], ot[i])
        P.skip = False
        P.emit()
    return nc


_CACHE = {}


def kernel(**inputs):
    inp = {k_: np.asarray(v) for k_, v in inputs.items()}
    hc = _host_consts()
    hw = _host_weights(inp)
    nc = build(False)
    x = inp["x"]
    in_maps = []
    for c in range(NCORE):
        m = {"x": np.ascontiguousarray(x[c * SPC:(c + 1) * SPC])}
        m.update(hw)
        m.update(hc)
        in_maps.append(m)
    res = run_bass_kernel_spmd(nc, in_maps, core_ids=list(range(NCORE)))
    out = np.concatenate([r["out"] for r in res.results], axis=0)
    return out.astype(np.float32)
```
